# Optimizing a Trainium2 kernel written in Bass

```python
import math
import jax, jax.numpy as jnp
from jax import lax
import numpy as np

D_MODEL = 1024
BATCH = 4
SEQ = 4096
DEPTH = 1

HEAD_DIM = 64
HEADS_A = 8
HEADS_B = 8
WIDTH_A = HEADS_A * HEAD_DIM
WIDTH_B = HEADS_B * HEAD_DIM
MIX_WIDTH = WIDTH_A + WIDTH_B
IN_WIDTH = 3 * WIDTH_A + 3 * WIDTH_B + HEADS_B
DILATIONS = ((128, 1), (512, 4), (2048, 16))
ROT_DIM = HEAD_DIM // 4
ROPE_THETA = 500000.0
Q_BLOCK = 128
N_GROUPS = 4
EXPERTS_PER_GROUP = 4
N_EXPERTS = N_GROUPS * EXPERTS_PER_GROUP
TOP_K = 2
D_EXPERT = 512
NORM_EPS = 1e-6
NEG_INF = -1e30

kernel_name = "hybrid_dilated_fox_hiermoe_layer"


def rmsnorm(x, g):
    xf = x.astype(jnp.float32)
    y = xf * lax.rsqrt(jnp.mean(xf * xf, axis=-1, keepdims=True) + NORM_EPS)
    return (y * g.astype(jnp.float32)).astype(x.dtype)


def partial_rotary(t, seq_len):
    pos = jnp.arange(seq_len, dtype=jnp.float32)
    inv_freq = 1.0 / (ROPE_THETA ** (jnp.arange(0, ROT_DIM, 2, dtype=jnp.float32) / ROT_DIM))
    ang = pos[:, None] * inv_freq[None, :]
    ang = jnp.concatenate([ang, ang], axis=-1)[:, None, :]
    cos, sin = jnp.cos(ang).astype(t.dtype), jnp.sin(ang).astype(t.dtype)
    tr, tp = t[..., :ROT_DIM], t[..., ROT_DIM:]
    half = ROT_DIM // 2
    rot_half = jnp.concatenate([-tr[..., half:], tr[..., :half]], axis=-1)
    return jnp.concatenate([tr * cos + rot_half * sin, tp], axis=-1)


def dilated_branch(q, k, v, window, dilation):
    b, s_len, h, d = q.shape
    w = window // dilation
    sub_len = s_len // dilation
    nb = -(-sub_len // w)
    pad_len = nb * w

    def split(t):
        t = t.reshape(b, sub_len, dilation, h, d).transpose(0, 2, 3, 1, 4)
        t = jnp.pad(t, ((0, 0), (0, 0), (0, 0), (0, pad_len - sub_len), (0, 0)))
        return t.reshape(b, dilation, h, nb, w, d)

    def with_prev(t):
        prev = jnp.pad(t[:, :, :, :-1], ((0, 0), (0, 0), (0, 0), (1, 0), (0, 0), (0, 0)))
        return jnp.concatenate([prev, t], axis=4)

    qb = split(q)
    kk, vv = with_prev(split(k)), with_prev(split(v))
    sc = jnp.einsum('brhnqd,brhnkd->brhnqk', qb, kk, preferred_element_type=jnp.float32)
    qi = jnp.arange(w)[:, None]
    kj = jnp.arange(2 * w)[None, :]
    dist = qi + w - kj
    band = (dist >= 0) & (dist <= w)
    valid_prev = (jnp.arange(nb)[:, None, None] > 0) | (kj[None] >= w)
    mask = band[None] & valid_prev
    sc = jnp.where(mask, sc, NEG_INF)
    m = jnp.max(sc, axis=-1, keepdims=True)
    p = jnp.exp(sc - m)
    l = jnp.sum(p, axis=-1, keepdims=True)
    o = jnp.einsum('brhnqk,brhnkd->brhnqd', (p / l).astype(v.dtype), vv)
    lse = (m + jnp.log(l))[..., 0]
    o = o.reshape(b, dilation, h, pad_len, d)[:, :, :, :sub_len]
    o = o.transpose(0, 3, 1, 2, 4).reshape(b, s_len, h, d)
    lse = lse.reshape(b, dilation, h, pad_len)[..., :sub_len]
    lse = lse.transpose(0, 3, 1, 2).reshape(b, s_len, h)
    return o, lse


def dilated_attention(q, k, v):
    outs, lses = [], []
    for window, dilation in DILATIONS:
        o, lse = dilated_branch(q, k, v, window, dilation)
        outs.append(o)
        lses.append(lse)
    wts = jax.nn.softmax(jnp.stack(lses, axis=0), axis=0)
    o = jnp.sum(wts[..., None] * jnp.stack(outs, axis=0).astype(jnp.float32), axis=0)
    return o.astype(q.dtype)


def forgetting_attention(q, k, v, log_f):
    b, s_len, h, d = q.shape
    c = jnp.cumsum(log_f, axis=1).transpose(0, 2, 1)
    qh, kh, vh = (t.transpose(0, 2, 1, 3) for t in (q, k, v))
    pos = jnp.arange(s_len)
    n_blocks = s_len // Q_BLOCK

    def block(i):
        start = i * Q_BLOCK
        qb = lax.dynamic_slice_in_dim(qh, start, Q_BLOCK, axis=2)
        cq = lax.dynamic_slice_in_dim(c, start, Q_BLOCK, axis=2)
        sc = jnp.einsum('bhqd,bhkd->bhqk', qb, kh, preferred_element_type=jnp.float32)
        sc = sc + cq[..., :, None] - c[:, :, None, :]
        qpos = start + jnp.arange(Q_BLOCK)
        sc = jnp.where(pos[None, :] <= qpos[:, None], sc, NEG_INF)
        p = jax.nn.softmax(sc, axis=-1)
        return jnp.einsum('bhqk,bhkd->bhqd', p.astype(vh.dtype), vh)

    o = lax.map(block, jnp.arange(n_blocks))
    return o.transpose(1, 0, 3, 2, 4).reshape(b, s_len, h, d)


def hierarchical_moe(h, w_group, w_expert, w_gate_e, w_up_e, w_down_e):
    b, s_len, dm = h.shape
    n = b * s_len
    h2 = h.reshape(n, dm)
    g_logits = jnp.einsum('nd,dg->ng', h2, w_group).astype(jnp.float32)
    p_group = jax.nn.softmax(g_logits, axis=-1)
    g_star = jnp.argmax(g_logits, axis=-1)
    p_top = jnp.take_along_axis(p_group, g_star[:, None], axis=1)[:, 0]
    e_logits = jnp.einsum('nd,de->ne', h2, w_expert).astype(jnp.float32)
    e_logits = e_logits.reshape(n, N_GROUPS, EXPERTS_PER_GROUP)
    sel = jnp.take_along_axis(e_logits, g_star[:, None, None], axis=1)[:, 0]
    top_v, top_i = lax.top_k(sel, TOP_K)
    wk = jax.nn.softmax(top_v, axis=-1) * p_top[:, None]
    eid = g_star[:, None] * EXPERTS_PER_GROUP + top_i
    comb = jnp.sum(jax.nn.one_hot(eid, N_EXPERTS, dtype=jnp.float32) * wk[..., None], axis=1)
    comb = comb.astype(h2.dtype)
    y = jnp.zeros_like(h2)
    for e in range(N_EXPERTS):
        he = jax.nn.silu(h2 @ w_gate_e[e]) * (h2 @ w_up_e[e])
        y = y + comb[:, e:e + 1] * (he @ w_down_e[e])
    return y.reshape(b, s_len, dm)


def setup_inputs(seed: int = 0) -> dict:
    key = jax.random.key(seed)
    ks = jax.random.split(key, 16)
    f32 = jnp.float32
    x = jax.random.normal(ks[0], (BATCH, SEQ, D_MODEL), f32)
    attn_norm = 1.0 + 0.05 * jax.random.normal(ks[1], (DEPTH, D_MODEL), f32)
    w_in = jax.random.normal(ks[2], (DEPTH, D_MODEL, IN_WIDTH), f32) * D_MODEL ** -0.5
    b_forget = 4.0 + 0.5 * jax.random.normal(ks[3], (DEPTH, HEADS_B), f32)
    w_out = jax.random.normal(ks[4], (DEPTH, MIX_WIDTH, D_MODEL), f32) * MIX_WIDTH ** -0.5
    ffn_norm = 1.0 + 0.05 * jax.random.normal(ks[5], (DEPTH, D_MODEL), f32)
    w_group = jax.random.normal(ks[6], (DEPTH, D_MODEL, N_GROUPS), f32) * D_MODEL ** -0.5
    w_expert = jax.random.normal(ks[7], (DEPTH, D_MODEL, N_EXPERTS), f32) * D_MODEL ** -0.5
    w_gate_e = jax.random.normal(ks[8], (DEPTH, N_EXPERTS, D_MODEL, D_EXPERT), f32) * D_MODEL ** -0.5
    w_up_e = jax.random.normal(ks[9], (DEPTH, N_EXPERTS, D_MODEL, D_EXPERT), f32) * D_MODEL ** -0.5
    w_down_e = jax.random.normal(ks[10], (DEPTH, N_EXPERTS, D_EXPERT, D_MODEL), f32) * D_EXPERT ** -0.5
    final_norm = 1.0 + 0.05 * jax.random.normal(ks[11], (D_MODEL,), f32)
    return {"x": x, "attn_norm": attn_norm, "w_in": w_in, "b_forget": b_forget,
            "w_out": w_out, "ffn_norm": ffn_norm, "w_group": w_group,
            "w_expert": w_expert, "w_gate_e": w_gate_e, "w_up_e": w_up_e,
            "w_down_e": w_down_e, "final_norm": final_norm}


def reference(x, attn_norm, w_in, b_forget, w_out, ffn_norm, w_group, w_expert,
              w_gate_e, w_up_e, w_down_e, final_norm):
    b, s_len, _ = x.shape
    scale = HEAD_DIM ** -0.5
    offs = np.cumsum([0, WIDTH_A, WIDTH_A, WIDTH_A, WIDTH_B, WIDTH_B, WIDTH_B, HEADS_B])
    for layer in range(DEPTH):
        h = rmsnorm(x, attn_norm[layer])
        proj = jnp.einsum('bsd,de->bse', h, w_in[layer])
        parts = [proj[..., offs[i]:offs[i + 1]] for i in range(7)]
        qa, ka, va = (t.reshape(b, s_len, HEADS_A, HEAD_DIM) for t in parts[0:3])
        qb, kb, vb = (t.reshape(b, s_len, HEADS_B, HEAD_DIM) for t in parts[3:6])
        qa = partial_rotary(qa, s_len) * scale
        ka = partial_rotary(ka, s_len)
        out_a = dilated_attention(qa, ka, va)
        log_f = jax.nn.log_sigmoid(parts[6].astype(jnp.float32) + b_forget[layer].astype(jnp.float32))
        out_b = forgetting_attention(qb * scale, kb, vb, log_f)
        mixed = jnp.concatenate([out_a.reshape(b, s_len, WIDTH_A),
                                 out_b.reshape(b, s_len, WIDTH_B)], axis=-1)
        x = x + jnp.einsum('bse,ed->bsd', mixed, w_out[layer])
        h2 = rmsnorm(x, ffn_norm[layer])
        x = x + hierarchical_moe(h2, w_group[layer], w_expert[layer], w_gate_e[layer],
                                 w_up_e[layer], w_down_e[layer])
    return rmsnorm(x, final_norm)
```

```python
import contextlib
import numpy as np
import concourse.bass as bass
import concourse.mybir as mybir
from concourse.bass_utils import run_bass_kernel_spmd

F32 = mybir.dt.float32
BF16 = mybir.dt.bfloat16
ALU = mybir.AluOpType
AF = mybir.ActivationFunctionType
AX = mybir.AxisListType

ENGS = ("pe", "act", "dve", "pool", "sp")
D = 1024
S_ALL = 4096
S_OWN = 2048
EPS = 1e-6
BIG = 1e30


class Op:
    __slots__ = ("eng", "fn", "reads", "writes", "is_dma", "deps", "signal", "sem", "val")

    def __init__(self, eng, fn, reads, writes, is_dma):
        self.eng = eng
        self.fn = fn
        self.reads = reads
        self.writes = writes
        self.is_dma = is_dma
        self.deps = []
        self.signal = False
        self.sem = None
        self.val = None


class Sched:
    def __init__(self, n_dma_sems=32):
        self.ops = {e: [] for e in ENGS}
        self.last_writer = {}
        self.readers = {}
        self.n_dma_sems = n_dma_sems
        self.dma_rr = 0
        self.dma_rr_pool = 0
        self.dma_last = [None] * n_dma_sems
        self.dma_count = [0] * n_dma_sems
        self.last_on = {e: None for e in ENGS}
        self.pending_fence = {e: [] for e in ENGS}

    def _dep(self, op, prod):
        if prod is None or prod is op:
            return
        if (not prod.is_dma) and (not op.is_dma) and prod.eng == "pe" and op.eng == "pe":
            return
        op.deps.append(prod)

    def add(self, eng, fn, reads=(), writes=(), is_dma=False):
        op = Op(eng, fn, tuple(reads), tuple(writes), is_dma)
        if self.pending_fence[eng]:
            for p in self.pending_fence[eng]:
                if p is not None and not (p.eng == "pe" and eng == "pe" and not p.is_dma and not is_dma):
                    op.deps.append(p)
            self.pending_fence[eng] = []
        for k in op.reads:
            self._dep(op, self.last_writer.get(k))
        for k in op.writes:
            self._dep(op, self.last_writer.get(k))
            for r in self.readers.get(k, ()):
                self._dep(op, r)
        for k in op.reads:
            self.readers.setdefault(k, []).append(op)
        for k in op.writes:
            self.last_writer[k] = op
            self.readers[k] = []
        if is_dma:
            half = self.n_dma_sems // 2
            if eng == "pool":
                k = half + self.dma_rr_pool
                self.dma_rr_pool = (self.dma_rr_pool + 1) % (self.n_dma_sems - half)
            else:
                k = self.dma_rr
                self.dma_rr = (self.dma_rr + 1) % half
            prev = self.dma_last[k]
            if prev is not None:
                op.deps.append(prev)
            self.dma_last[k] = op
            self.dma_count[k] += 1
            op.sem = ("dma", k)
            op.val = 16 * self.dma_count[k]
            op.signal = True
        else:
            self.last_on[eng] = op
        self.ops[eng].append(op)
        return op

    def fence(self):
        prods = [self.last_on[e] for e in ENGS] + list(self.dma_last)
        for e in ENGS:
            self.pending_fence[e] = list(prods)
        self.last_writer = {}
        self.readers = {}

    def emit(self, nc, final_wait_ops=()):
        for e in ENGS:
            for op in self.ops[e]:
                for p in op.deps:
                    if not p.is_dma:
                        p.signal = True
        for e in ENGS:
            c = 0
            for op in self.ops[e]:
                if op.is_dma:
                    continue
                if op.signal:
                    c += 1
                    op.sem = ("eng", e)
                    op.val = c
        with contextlib.ExitStack() as es:
            sems = {}
            for e in ENGS:
                sems[("eng", e)] = es.enter_context(nc.semaphore("s_" + e))
            for k in range(self.n_dma_sems):
                sems[("dma", k)] = es.enter_context(nc.semaphore("s_dma%d" % k))
            block = es.enter_context(nc.Block())
            engmap = {"pe": "tensor", "act": "scalar", "dve": "vector",
                      "pool": "gpsimd", "sp": "sync"}

            def make(e):
                def body(engine):
                    waited = {}
                    for op in self.ops[e]:
                        need = {}
                        for p in op.deps:
                            if p.sem is None:
                                continue
                            if need.get(p.sem, 0) < p.val:
                                need[p.sem] = p.val
                        for s, v in need.items():
                            if waited.get(s, 0) >= v:
                                continue
                            engine.wait_ge(sems[s], v)
                            waited[s] = v
                        ins = op.fn(engine)
                        if op.signal:
                            ins.then_inc(sems[op.sem], 16 if op.is_dma else 1)
                    if e == "sp":
                        for p in final_wait_ops:
                            engine.wait_ge(sems[p.sem], p.val)
                return body

            for e in ENGS:
                getattr(block, engmap[e])(make(e))


class _Stop(Exception):
    pass


def build_nc(debug=False, limit=99):
    nc = bass.Bass("TRN2", target_bir_lowering=False)

    def din(name, shape):
        return nc.dram_tensor(name, shape, F32, kind="ExternalInput").ap()

    xkv = din("xkv", [S_ALL, D])
    w_in = din("w_in", [D, 3080])
    w_out = din("w_out", [D, D])
    attn_norm = din("attn_norm", [1, D])
    ffn_norm = din("ffn_norm", [1, D])
    final_norm = din("final_norm", [1, D])
    b_forget = din("b_forget", [8, 1])
    w_route = din("w_route", [D, 20])
    w_gate = din("w_gate", [16, D, 512])
    w_up = din("w_up", [16, D, 512])
    w_down = din("w_down", [16, 512, D])
    cosT = din("cosT", [128, S_ALL])
    sinT = din("sinT", [128, S_ALL])
    valid_d = din("valid", [128, 84])
    maskT_d = din("maskT", [128, 896])
    maskPC_d = din("maskPC", [128, 512])
    ident_d = din("ident", [128, 128])
    rotR_d = din("rotR", [128, 128])
    out_d = nc.dram_tensor("out", [S_OWN, D], F32, kind="ExternalOutput").ap()
    aug_d = nc.dram_tensor("aug_scratch", [4, 8, S_ALL], BF16, kind="Internal").ap()
    dbg = {}
    if debug:
        dbg["mixT"] = nc.dram_tensor("dbg_mixT", [128, 8 * S_OWN], F32, kind="ExternalOutput").ap()
        dbg["x1"] = nc.dram_tensor("dbg_x1", [128, 16 * D], F32, kind="ExternalOutput").ap()
        dbg["comb"] = nc.dram_tensor("dbg_comb", [128, 16 * 16], F32, kind="ExternalOutput").ap()

    S = Sched()
    out_ops = []

    def op(eng, method, reads, writes, *args, **kwargs):
        return S.add(eng, lambda e: getattr(e, method)(*args, **kwargs), reads, writes)

    def dma(eng, out, in_, reads, writes):
        return S.add(eng, lambda e: e.dma_start(out=out, in_=in_), reads, writes, is_dma=True)

    def mm(out, lhsT, rhs, start, stop, reads, writes):
        return S.add("pe", lambda e: e.matmul(out, lhsT=lhsT, rhs=rhs, start=start, stop=stop), reads, writes)

    def act(out, in_, func, reads, writes, **kw):
        return S.add("act", lambda e: e.activation(out=out, in_=in_, func=func, **kw), reads, writes)

    def checkpoint(n):
        if limit == n:
            S.fence()
            out_ops.append(dma("sp", out_d[0:128, :], gnb_holder[0][:], [], []))
            raise _Stop()

    gnb_holder = []
    try:
      with contextlib.ExitStack() as top:
        def sbt(stack, name, shape, dt):
            return stack.enter_context(nc.sbuf_tensor("sb_" + name, shape, dt))

        ps_s = [top.enter_context(nc.psum_tensor("ps_s%d" % i, [128, 1024], F32)) for i in range(2)]
        ps_o = [top.enter_context(nc.psum_tensor("ps_o%d" % i, [128, 512], F32)) for i in range(2)]
        ps_m = top.enter_context(nc.psum_tensor("ps_m", [128, 512], F32))
        ps_t = top.enter_context(nc.psum_tensor("ps_t", [128, 1024], BF16))
        banks = [(ps_s[0][:, 0:512], "b0"), (ps_s[0][:, 512:1024], "b1"),
                 (ps_s[1][:, 0:512], "b2"), (ps_s[1][:, 512:1024], "b3"),
                 (ps_o[0][:, :], "b4"), (ps_o[1][:, :], "b5"), (ps_m[:, :], "b6")]
        PS_S_KEYS = [("b0", "b1"), ("b2", "b3")]
        PS_O_KEYS = ["b4", "b5"]
        bank_rr = [0]

        bank_override = [None]
        bank7 = (ps_t[:, :].bitcast(F32), "ps_t")

        def next_bank(n=4):
            if bank_override[0] is not None:
                lst = bank_override[0]
                i = bank_rr[0] % len(lst)
                bank_rr[0] += 1
                return lst[i]
            i = bank_rr[0] % n
            bank_rr[0] += 1
            return banks[i]

        ident32 = sbt(top, "ident32", [128, 128], F32)
        ident16 = sbt(top, "ident16", [128, 128], BF16)
        maskT16 = sbt(top, "maskT16", [128, 896], BF16)
        maskPC16 = sbt(top, "maskPC16", [128, 512], BF16)
        valid = sbt(top, "valid", [128, 84], F32)
        gnb = sbt(top, "gnb", [128, D], F32)
        gnb_holder.append(gnb)
        mixT = sbt(top, "mixT", [128, 8, S_OWN], BF16)
        ss = sbt(top, "ss", [128, 32], F32)
        rstd = sbt(top, "rstd", [128, 32], F32)

        dma("sp", ident32[:], ident_d, [], ["ident32"])
        op("dve", "tensor_copy", ["ident32"], ["ident16"], out=ident16[:], in_=ident32[:])
        dma("pool", maskT16[:], maskT_d, [], ["maskT16"])
        dma("pool", maskPC16[:], maskPC_d, [], ["maskPC16"])
        dma("sp", valid[:], valid_d, [], ["valid"])
        dma("sp", gnb[:], attn_norm.partition_broadcast(128), [], ["gnb"])
        mones = sbt(top, "mones", [128, 2], F32)
        op("pool", "memset", [], ["mones"], mones[:], -1.0)
        ones8 = sbt(top, "ones8", [8, 2], F32)
        op("pool", "memset", [], ["ones8"], ones8[:], 1.0)
        ones32r = sbt(top, "ones32r", [128, 128], mybir.dt.float32r)
        act(ones32r[:, :], ident32[:, :], AF.Identity, ["ident32"], ["ones32r"], scale=0.0, bias=1.0)

        def rms_stats(src_ap, col, srckeys, junk_ap):
            act(junk_ap, src_ap, AF.Square, srckeys, ["junk", ("ss", col)], accum_out=ss[:, col:col + 1])
            act(rstd[:, col:col + 1], ss[:, col:col + 1], AF.Sqrt, [("ss", col)], [("rstd", col)],
                scale=1.0 / D, bias=EPS)
            op("dve", "reciprocal", [("rstd", col)], [("rstd", col)], out=rstd[:, col:col + 1], in_=rstd[:, col:col + 1])

        with contextlib.ExitStack() as att:
            hT = sbt(att, "hT", [128, 8, S_ALL], BF16)
            wqB = sbt(att, "wqB", [128, 8, 128], BF16)
            wkB = sbt(att, "wkB", [128, 8, 128], BF16)
            wvB = sbt(att, "wvB", [128, 8, 128], BF16)
            wf16 = sbt(att, "wf16", [128, 8, 8], BF16)
            for (dst_, col_, n_, key_) in ((wf16, 3072, 8, "wf16"), (wkB, 2048, 128, ("wkB", 0)), (wqB, 1536, 128, ("wqB", 0)),
                                           (wvB, 2560, 128, ("wvB", 0))):
                dma("pool", dst_[:, :, 0:n_], w_in[:, col_:col_ + n_].rearrange("(kc p) n -> p kc n", p=128), [], [key_])
            with contextlib.ExitStack() as p0:
                xbuf = [sbt(p0, "xbuf%d" % i, [128, D], F32) for i in range(6)]
                junk16 = sbt(p0, "junk16", [128, D], BF16)
                xn16 = [sbt(p0, "xn16_%d" % i, [128, D], BF16) for i in range(2)]
                def p0_s1(tb):
                    xt = xbuf[tb % 6]
                    xk = ("xbuf", tb % 6)
                    dma("sp", xt[:], xkv[tb * 128:(tb + 1) * 128, :], [], [xk])
                    rms_stats(xt[:], tb, [xk], junk16[:])

                def p0_s2(tb):
                    xt = xbuf[tb % 6]
                    xk = ("xbuf", tb % 6)
                    xn = xn16[tb % 2]
                    nk = ("xn16", tb % 2)
                    op("dve", "scalar_tensor_tensor", [xk, ("rstd", tb), "gnb"], [nk], out=xn[:], in0=xt[:],
                       scalar=rstd[:, tb:tb + 1], in1=gnb[:], op0=ALU.mult, op1=ALU.mult)
                    tbank = ps_t[:, :] if tb % 2 == 0 else ps_m[:, :].bitcast(BF16)
                    tkey = "ps_t" if tb % 2 == 0 else "b6"
                    for kc in range(8):
                        S.add("pe", (lambda kc=kc, xn=xn, tbank=tbank: (lambda e: e.transpose(tbank[:, kc * 128:(kc + 1) * 128],
                                                                                              xn[:, kc * 128:(kc + 1) * 128], ident16[:])))(),
                              [nk, "ident16"], [tkey])

                def p0_s3(tb):
                    tbank = ps_t[:, :] if tb % 2 == 0 else ps_m[:, :].bitcast(BF16)
                    tkey = "ps_t" if tb % 2 == 0 else "b6"
                    src = tbank.rearrange("p (k t) -> p k t", k=8)
                    dst = hT[:, :, tb * 128:(tb + 1) * 128]
                    if tb % 2 == 0:
                        act(dst, src, AF.Copy, [tkey], [("hT", tb // 4, 0)])
                    else:
                        op("dve", "tensor_copy", [tkey], [("hT", tb // 4, 1)], out=dst, in_=src)

                for i in range(32 + 3):
                    if i < 32:
                        p0_s1(i)
                    if 1 <= i <= 32:
                        p0_s2(i - 1)
                    if 2 <= i - 1 + 1 and 0 <= i - 2 < 32:
                        p0_s3(i - 2)
            S.fence()
            checkpoint(1)

            def hT_keys(chunk):
                return [("hT", chunk, 0), ("hT", chunk, 1)]

            def load_w(dst, col0, ncols, key):
                dma("pool", dst[:, :, 0:ncols],
                    w_in[:, col0:col0 + ncols].rearrange("(kc p) n -> p kc n", p=128), [], [key])

            def proj_fm(wt, wkey, chunk, ncols=128):
                bk, bkey = next_bank()
                for kc in range(8):
                    mm(bk[0:ncols, :], wt[:, kc, 0:ncols], hT[:, kc, chunk * 512:(chunk + 1) * 512],
                       kc == 0, kc == 7, [wkey] + hT_keys(chunk), [bkey])
                return bk, bkey

            def proj_v(wv, vp, tiles, wkey="wv", tag=0):
                for g0 in range(0, len(tiles), 4):
                    grp = tiles[g0:g0 + 4]
                    bk, bkey = next_bank()
                    for i, (vi, tsl, ck) in enumerate(grp):
                        for kc in range(8):
                            mm(bk[:, i * 128:(i + 1) * 128], hT[:, kc, tsl], wv[:, kc, :],
                               kc == 0, kc == 7, [wkey] + ck, [bkey])
                    n = len(grp)
                    vi0 = grp[0][0]
                    assert all(grp[i][0] == vi0 + i for i in range(n))
                    src = bk[:, 0:n * 128].rearrange("p (t c) -> p t c", t=n)
                    act(vp[:, vi0:vi0 + n, 0:64], src[:, :, 0:64], AF.Copy, [bkey], [("vp", tag, vi0, 0)])
                    act(vp[:, vi0:vi0 + n, 130:194], src[:, :, 64:128], AF.Copy, [bkey], [("vp", tag, vi0, 1)])

            def vp_keys(vi, tag=0):
                v0 = (vi // 4) * 4
                return [("vp", tag, v0, 0), ("vp", tag, v0, 1), ("vp_init", tag)]

            def init_vp(vp, ntiles, tag=0):
                vk = ("vp_init", tag)
                op("pool", "memset", [], [vk], vp[:, :, :], 0.0)
                op("dve", "tensor_copy", ["valid", vk], [vk], out=vp[:, :, 64:65],
                   in_=valid[:, 0:ntiles].unsqueeze(2))
                op("dve", "tensor_copy", ["valid", vk], [vk], out=vp[:, :, 66:67],
                   in_=valid[:, 0:ntiles].unsqueeze(2))

            def norm_a(hh, Ua, ua_cols, uakeys, recs, g2):
                row_s = 64 if hh == 0 else 0
                rec32, rhi, rlo = recs
                rs = slice(row_s, row_s + 1)
                op("pool", "tensor_tensor", uakeys + ["mones"], [("rec32", g2)], out=rec32[g2][rs, :],
                   in0=Ua[rs, ua_cols], in1=mones[rs, 0:1].to_broadcast([1, 512]), op=ALU.pow)

            def norm_b(hh, Ua, ua_cols, uakeys, recs, g2, pair_idx, j):
                row_s = 64 if hh == 0 else 0
                r0 = 0 if hh == 0 else 64
                M = 64 if hh == 0 else 128
                rec32, rhi, rlo = recs
                rs = slice(row_s, row_s + 1)
                mm(ps_m[0:M, :], ones32r[rs, 0:M], rec32[g2][rs, :], True, True, ["ones32r", ("rec32", g2)], ["b6"])
                op("dve", "tensor_tensor", uakeys + ["b6"], [("mixT", pair_idx, j)],
                   out=mixT[r0:r0 + 64, pair_idx, j * 512:(j + 1) * 512], in0=Ua[r0:r0 + 64, ua_cols],
                   in1=ps_m[r0:r0 + 64, :], op=ALU.mult)

            pending = []
            gstep = [0]

            def flush_pending(all_=False):
                pending.sort(key=lambda t: t[0])
                while pending and (all_ or pending[0][0] <= gstep[0]):
                    pending.pop(0)[1]()

            with contextlib.ExitStack() as pb:
                bf = sbt(pb, "bf", [8, 1], F32)
                nbf = sbt(pb, "nbf", [8, 1], F32)
                with contextlib.ExitStack() as pc:
                    chi = sbt(pc, "chi", [8, S_ALL], BF16)
                    clo = sbt(pc, "clo", [8, S_ALL], BF16)
                    nhi = sbt(pc, "nhi", [8, S_OWN], BF16)
                    nlo = sbt(pc, "nlo", [8, S_OWN], BF16)
                    c2 = sbt(pc, "c2", [8, S_ALL], F32)
                    c32 = sbt(pc, "c32", [8, S_ALL], F32)
                    dma("sp", bf[:], b_forget, [], ["bf"])
                    op("dve", "tensor_scalar", ["bf"], ["nbf"], out=nbf[:], in0=bf[:], scalar1=-1.0, scalar2=None,
                       op0=ALU.mult)
                    for chunk in range(8):
                        bk, bkey = proj_fm(wf16, "wf16", chunk, ncols=8)
                        act(c2[:, chunk * 512:(chunk + 1) * 512], bk[0:8, :], AF.Exp, [bkey, "nbf"], [("c2", chunk)],
                            scale=-1.0, bias=nbf[:, 0:1])
                    act(c2[:, :], c2[:, :], AF.Ln, [("c2", c) for c in range(8)], ["c2all"], bias=1.0)
                    op("dve", "tensor_tensor_scan", ["c2all", "ones8"], ["c32"], out=c32[:, :],
                       data0=ones8[0:8, 0:1].to_broadcast([8, S_ALL]), data1=c2[:, :], initial=0.0,
                       op0=ALU.mult, op1=ALU.add)
                    op("dve", "tensor_copy", ["c32"], ["chi"], out=chi[:, :], in_=c32[:, :])
                    op("dve", "tensor_tensor", ["c32", "chi"], ["clo"], out=clo[:, :], in0=c32[:, :], in1=chi[:, :],
                       op=ALU.subtract)
                    op("dve", "tensor_scalar", ["chi"], ["nhi"], out=nhi[:, :], in0=chi[:, S_OWN:S_ALL], scalar1=-1.0,
                       scalar2=None, op0=ALU.mult)
                    op("dve", "tensor_scalar", ["clo"], ["nlo"], out=nlo[:, :], in0=clo[:, S_OWN:S_ALL], scalar1=-1.0,
                       scalar2=None, op0=ALU.mult)
                    dma("sp", aug_d[0], chi[:, :], ["chi"], ["aug_d0"])
                    dma("sp", aug_d[1], clo[:, :], ["clo"], ["aug_d1"])
                    dma("sp", aug_d[2][:, 0:S_OWN], nhi[:, :], ["nhi"], ["aug_d2"])
                    dma("sp", aug_d[3][:, 0:S_OWN], nlo[:, :], ["nlo"], ["aug_d3"])
                S.fence()
                checkpoint(2)
                KTs = [[sbt(pb, "KT%d_%d" % (s_, i), [128, S_ALL], BF16) for i in range(2)] for s_ in range(2)]
                QTs = [[sbt(pb, "QT%d_%d" % (s_, i), [128, S_OWN], BF16) for i in range(2)] for s_ in range(2)]
                vps = [sbt(pb, "vpB%d" % s_, [128, 32, 194], BF16) for s_ in range(2)]
                Ua = [sbt(pb, "UaB%d" % i, [128, 512], F32) for i in range(2)]
                pt = [sbt(pb, "ptB%d" % i, [128, 1024], BF16) for i in range(2)]
                rec32B = [sbt(pb, "rec32B%d" % i, [128, 512], mybir.dt.float32r) for i in range(2)]
                recs = (rec32B, rec32B, rec32B)
                wsets = [(wqB, wkB, wvB),
                         (sbt(pb, "wqB2", [128, 8, 128], BF16), sbt(pb, "wkB2", [128, 8, 128], BF16),
                          sbt(pb, "wvB2", [128, 8, 128], BF16))]
                for s_ in range(2):
                    init_vp(vps[s_], 32, tag=s_)
                    op("pool", "memset", [], [("KT1z", s_)], KTs[s_][1][0:64, :], 0.0)
                    op("pool", "memset", [], [("QT1z", s_)], QTs[s_][1][0:64, :], 0.0)
                    op("pool", "memset", [], [("KT0a", s_)], KTs[s_][0][64:68, :], 1.0)
                    op("pool", "memset", [], [("QT0a", s_)], QTs[s_][0][64:68, :], 1.0)
                    op("pool", "memset", [("KT1z", s_)], [("KT1a", s_)], KTs[s_][1][0:4, :], 1.0)
                    op("pool", "memset", [("QT1z", s_)], [("QT1a", s_)], QTs[s_][1][0:4, :], 1.0)
                checkpoint(20)

                def emit_proj(pbi):
                    s_ = pbi % 2
                    wq, wk, wv = wsets[s_]
                    KT, QT, vp = KTs[s_], QTs[s_], vps[s_]
                    wqk, wkk, wvk = ("wqB", s_), ("wkB", s_), ("wvB", s_)
                    groups = []
                    if pbi > 0:
                        def g_load():
                            load_w(wk, 2048 + 128 * pbi, 128, wkk)
                            load_w(wq, 1536 + 128 * pbi, 128, wqk)
                            load_w(wv, 2560 + 128 * pbi, 128, wvk)
                        groups.append(g_load)
                    for chunk in range(8):
                        def g_k(chunk=chunk):
                            bk, bkey = proj_fm(wk, wkk, chunk)
                            act(KT[0][0:64, chunk * 512:(chunk + 1) * 512], bk[0:64, :], AF.Copy, [bkey], [("KT0", s_, chunk)])
                            op("dve", "tensor_copy", [bkey], [("KT1", s_, chunk)],
                               out=KT[1][64:128, chunk * 512:(chunk + 1) * 512], in_=bk[64:128, :])
                        groups.append(g_k)
                    for j in range(4):
                        def g_q(j=j):
                            bk, bkey = proj_fm(wq, wqk, 4 + j)
                            act(QT[0][0:64, j * 512:(j + 1) * 512], bk[0:64, :], AF.Copy, [bkey], [("QT0", s_, j)], scale=0.125)
                            op("dve", "tensor_scalar", [bkey], [("QT1", s_, j)], out=QT[1][64:128, j * 512:(j + 1) * 512],
                               in0=bk[64:128, :], scalar1=0.125, scalar2=None, op0=ALU.mult)
                        groups.append(g_q)
                    for g0 in range(0, 32, 4):
                        def g_v(g0=g0):
                            proj_v(wv, vp, [(blk, slice(blk * 128, (blk + 1) * 128), hT_keys(blk // 4))
                                            for blk in range(g0, g0 + 4)], wkey=wvk, tag=s_)
                        groups.append(g_v)

                    def g_aug():
                        h0, h1 = 2 * pbi, 2 * pbi + 1
                        dma("sp", KT[0][66:67, :], aug_d[0][h0:h0 + 1, :], ["aug_d0", ("KT0a", s_)], [("KT0aug", s_)])
                        dma("sp", KT[0][67:68, :], aug_d[1][h0:h0 + 1, :], ["aug_d1", ("KT0a", s_)], [("KT0aug2", s_)])
                        dma("sp", QT[0][64:65, :], aug_d[2][h0:h0 + 1, 0:S_OWN], ["aug_d2", ("QT0a", s_)], [("QT0aug", s_)])
                        dma("sp", QT[0][65:66, :], aug_d[3][h0:h0 + 1, 0:S_OWN], ["aug_d3", ("QT0a", s_)], [("QT0aug2", s_)])
                        dma("sp", KT[1][2:3, :], aug_d[0][h1:h1 + 1, :], ["aug_d0", ("KT1a", s_)], [("KT1aug", s_)])
                        dma("sp", KT[1][3:4, :], aug_d[1][h1:h1 + 1, :], ["aug_d1", ("KT1a", s_)], [("KT1aug2", s_)])
                        dma("sp", QT[1][0:1, :], aug_d[2][h1:h1 + 1, 0:S_OWN], ["aug_d2", ("QT1a", s_)], [("QT1aug", s_)])
                        dma("sp", QT[1][1:2, :], aug_d[3][h1:h1 + 1, 0:S_OWN], ["aug_d3", ("QT1a", s_)], [("QT1aug2", s_)])
                    groups.insert(1 if pbi > 0 else 0, g_aug)
                    return groups

                def fox_qk(st, it, s_):
                    hh, j, kb, nkb, g = st
                    kt, qt = KTs[s_][hh], QTs[s_][hh]
                    rows = slice(0, 68) if hh == 0 else slice(0, 128)
                    kaug = [("KT%daug" % hh, s_), ("KT%daug2" % hh, s_), ("KT%da" % hh, s_)] + ([("KT1z", s_)] if hh else [])
                    qaug = [("QT%daug" % hh, s_), ("QT%daug2" % hh, s_), ("QT%da" % hh, s_)] + ([("QT1z", s_)] if hh else [])
                    psb = ps_s[it % 2]
                    pskeys = PS_S_KEYS[it % 2]
                    for u in range(2):
                        k = kb + u
                        dg = k - (16 + 4 * j)
                        if dg >= 0:
                            off = 384 - 128 * dg
                            mm(psb[:, u * 512:(u + 1) * 512], ident16[:, :], maskT16[:, off:off + 512],
                               True, False, ["ident16", "maskT16"], [pskeys[u]])
                        mm(psb[:, u * 512:(u + 1) * 512], kt[rows, k * 128:(k + 1) * 128],
                           qt[rows, j * 512:(j + 1) * 512], dg < 0, True,
                           [("KT%d" % hh, s_, k // 4), ("QT%d" % hh, s_, j)] + kaug + qaug, [pskeys[u]])
                    act(pt[it % 2][:, :], psb[:, :], AF.Exp, list(pskeys), [("pt", it % 2)])

                def fox_pv(st, it, s_, g_base):
                    hh, j, kb, nkb, g = st
                    vp = vps[s_]
                    vcols = slice(0, 65) if hh == 0 else slice(66, 194)
                    M = 65 if hh == 0 else 128
                    gg = g_base + g
                    po = ps_o[gg % 2]
                    pokey = PS_O_KEYS[gg % 2]
                    ptb = pt[it % 2]
                    for u in range(2):
                        k = kb + u
                        mm(po[0:M, :], vp[:, k, vcols], ptb[:, u * 512:(u + 1) * 512], k == 0, k == nkb - 1,
                           vp_keys(k, s_) + [("pt", it % 2)], [pokey])
                    if kb + 2 >= nkb:
                        rsl = slice(0, 65) if hh == 0 else slice(0, 128)
                        op("dve", "tensor_copy", [pokey], [("Ua", gg % 2)], out=Ua[gg % 2][rsl, :], in_=po[rsl, :])
                        norm_a(hh, Ua[gg % 2], slice(0, 512), [("Ua", gg % 2)], recs, gg % 2)
                        return (hh, j, gg)
                    return None

                for g_ in emit_proj(0):
                    g_()
                it = 0
                for pbi in range(4):
                    s_ = pbi % 2
                    queue = emit_proj(pbi + 1) if pbi + 1 < 4 else []
                    steps = []
                    gidx = 0
                    for hh in range(2):
                        for j in range(4):
                            nkb = 16 + 4 * j + 4
                            for kb in range(0, nkb, 2):
                                steps.append((hh, j, kb, nkb, gidx))
                            gidx += 1
                    g_base = 8 * pbi
                    n = len(steps)
                    for i in range(n + 1):
                        if i < n:
                            fox_qk(steps[i], it + i, s_)
                        if i >= 1:
                            fin = fox_pv(steps[i - 1], it + i - 1, s_, g_base)
                            if fin is not None:
                                hh_, j_, gg_ = fin
                                pending.append((gstep[0] + 5, (lambda hh_=hh_, j_=j_, gg_=gg_, pbi=pbi: norm_b(
                                    hh_, Ua[gg_ % 2], slice(0, 512), [("Ua", gg_ % 2)], recs, gg_ % 2, 4 + pbi, j_))))
                        if i >= 2 and queue:
                            bank_override[0] = [bank7, banks[6]]
                            queue.pop(0)()
                            bank_override[0] = None
                        gstep[0] += 1
                        flush_pending()
                    while queue:
                        queue.pop(0)()
                    it += n
                flush_pending(all_=True)
            S.fence()
            checkpoint(3)

            with contextlib.ExitStack() as pa:
                KTa = sbt(pa, "KTa", [128, S_ALL], BF16)
                QTa = sbt(pa, "QTa", [128, S_OWN], BF16)
                vp = sbt(pa, "vpA", [128, 84, 194], BF16)
                Ua = [sbt(pa, "UaA%d" % i, [128, S_OWN], F32) for i in range(2)]
                pt = [sbt(pa, "ptA%d" % i, [128, 1024], BF16) for i in range(2)]
                rec32A = [sbt(pa, "rec32A%d" % i, [128, 512], mybir.dt.float32r) for i in range(4)]
                recs = (rec32A, rec32A, rec32A)
                _wA = tuple(sbt(pa, "w%sA" % n_, [128, 8, 128], BF16) for n_ in ("q", "k", "v"))
                wsetsA = [_wA, _wA]

                def load_wA(pai_):
                    s_ = pai_ % 2
                    load_w(wsetsA[s_][1], 512 + 128 * pai_, 128, ("wkA", 0))
                    load_w(wsetsA[s_][0], 0 + 128 * pai_, 128, ("wqA", 0))
                    load_w(wsetsA[s_][2], 1024 + 128 * pai_, 128, ("wvA", 0))

                load_wA(0)
                cst = [sbt(pa, "cst%d" % i, [128, 512], F32) for i in range(2)]
                snt = [sbt(pa, "snt%d" % i, [128, 512], F32) for i in range(2)]
                tmp1 = sbt(pa, "tmp1", [128, 512], F32)
                tmp2 = sbt(pa, "tmp2", [128, 512], F32)
                init_vp(vp, 84)
                T16 = [sbt(pa, "T16_%d" % i, [128, 512], BF16) for i in range(2)]
                R16 = sbt(pa, "R16", [128, 128], BF16)
                dma("pool", R16[:, :], rotR_d, [], ["R16"])
                v_tiles = [(blk, slice(blk * 128, (blk + 1) * 128), hT_keys(blk // 4)) for blk in range(12, 32)]
                for r4 in range(4):
                    for m in range(3, 8):
                        v_tiles.append((32 + r4 * 5 + (m - 3), slice(512 * m + r4, 512 * (m + 1), 4), hT_keys(m)))
                for r16 in range(16):
                    for n in range(2):
                        v_tiles.append((52 + 2 * r16 + n, slice(2048 * n + r16, 2048 * (n + 1), 16),
                                        [k_ for c in range(4 * n, 4 * n + 4) for k_ in hT_keys(c)]))
                it = 0
                for pai in range(4):
                    wq, wk, wv = wsetsA[pai % 2]
                    wqk_, wkk_, wvk_ = ("wqA", 0), ("wkA", 0), ("wvA", 0)
                    ajobs = []
                    for chunk in range(8):
                        ajobs.append((chunk, True, wk, wkk_, KTa[:, chunk * 512:(chunk + 1) * 512], ("KTa", chunk), 1.0))
                        if chunk >= 4:
                            j = chunk - 4
                            ajobs.append((chunk, False, wq, wqk_, QTa[:, j * 512:(j + 1) * 512], ("QTa", j), 0.125))

                    def a_s1(n):
                        chunk, first, w, wkey, dst, dkey, scl = ajobs[n]
                        if first:
                            dma("sp", cst[chunk % 2][:], cosT[:, chunk * 512:(chunk + 1) * 512], [], [("cst", chunk % 2)])
                            dma("sp", snt[chunk % 2][:], sinT[:, chunk * 512:(chunk + 1) * 512], [], [("snt", chunk % 2)])
                        bT, bTk = proj_fm(w, wkey, chunk)
                        act(T16[n % 2][:, :], bT, AF.Copy, [bTk], [("T16", n % 2)])

                    def a_s2(n):
                        chunk, first, w, wkey, dst, dkey, scl = ajobs[n]
                        ct, st = cst[chunk % 2], snt[chunk % 2]
                        ck, sk = ("cst", chunk % 2), ("snt", chunk % 2)
                        t16, t16k = T16[n % 2], ("T16", n % 2)
                        bR, bRk = next_bank()
                        mm(bR, R16[:, :], t16[:, :], True, True, ["R16", t16k], [bRk])
                        op("dve", "scalar_tensor_tensor", [t16k, ck], ["tmp1"], out=tmp1[:], in0=t16[:, :], scalar=scl,
                           in1=ct[:], op0=ALU.mult, op1=ALU.mult)
                        op("dve", "scalar_tensor_tensor", [bRk, sk], ["tmp2"], out=tmp2[:], in0=bR, scalar=scl,
                           in1=st[:], op0=ALU.mult, op1=ALU.mult)
                        op("pool", "tensor_tensor", ["tmp1", "tmp2"], [dkey], out=dst, in0=tmp1[:], in1=tmp2[:],
                           op=ALU.add)

                    vgroups = [v_tiles[g0:g0 + 4] for g0 in range(0, len(v_tiles), 4)]
                    per_step = -(-len(vgroups) // (len(ajobs) + 1))
                    for n in range(len(ajobs) + 1):
                        if n < len(ajobs):
                            a_s1(n)
                        if n >= 1:
                            a_s2(n - 1)
                        for _ in range(per_step):
                            if vgroups:
                                proj_v(wv, vp, vgroups.pop(0), wkey=wvk_)
                    while vgroups:
                        proj_v(wv, vp, vgroups.pop(0), wkey=wvk_)
                    if pai + 1 < 4:
                        load_wA(pai + 1)
                    flush_pending(all_=True)
                    all_groups = []
                    for hh in range(2):
                        rows = slice(0, 64) if hh == 0 else slice(64, 128)
                        vcols = slice(0, 65) if hh == 0 else slice(66, 194)
                        M = 65 if hh == 0 else 128
                        rsl = slice(0, 65) if hh == 0 else slice(0, 128)
                        ua = Ua[hh]
                        uak = ("Ua", hh)
                        groups = []
                        for gq in range(4):
                            qbs = []
                            for qb in range(4 * gq, 4 * gq + 4):
                                qbs.append((slice(128 * qb, 128 * qb + 128),
                                            (slice(2048 + 128 * (qb - 1), 2048 + 128 * qb), 16 + qb - 1),
                                            (slice(2048 + 128 * qb, 2048 + 128 * (qb + 1)), 16 + qb)))
                            groups.append((qbs, ua[rsl, 512 * gq:512 * (gq + 1)], True))
                        for r4 in range(4):
                            qbs = []
                            for qm in range(4, 8):
                                qbs.append((slice(512 * (qm - 4) + r4, 512 * (qm - 3), 4),
                                            (slice(512 * (qm - 1) + r4, 512 * qm, 4), 32 + r4 * 5 + (qm - 1 - 3)),
                                            (slice(512 * qm + r4, 512 * (qm + 1), 4), 32 + r4 * 5 + (qm - 3))))
                            groups.append((qbs, ua[rsl, r4:S_OWN:4], False))
                        ua16 = ua[rsl, :].rearrange("p (i r) -> p r i", r=16)
                        for g in range(4):
                            qbs = []
                            for r16 in range(4 * g, 4 * g + 4):
                                qbs.append((slice(r16, 2048, 16),
                                            (slice(r16, 2048, 16), 52 + 2 * r16),
                                            (slice(2048 + r16, 4096, 16), 52 + 2 * r16 + 1)))
                            groups.append((qbs, ua16[:, 4 * g:4 * g + 4, :], False))
                        for (qbs, dst, first) in groups:
                            all_groups.append((hh, qbs, dst, first))

                    def a_front(gr, itx):
                        hh, qbs, dst, first = gr
                        rows = slice(0, 64) if hh == 0 else slice(64, 128)
                        psb = ps_s[itx % 2]
                        pskeys = PS_S_KEYS[itx % 2]
                        ptb = pt[itx % 2]
                        ptkey = ("pt", itx % 2)
                        for q, (qsl, prev, cur) in enumerate(qbs):
                            for u, (ksl, vi) in enumerate((prev, cur)):
                                col = (2 * q + u) * 128
                                mm(psb[:, col:col + 128], KTa[rows, ksl], QTa[rows, qsl], True, True,
                                   [("KTa", c) for c in range(8)] + [("QTa", c) for c in range(4)],
                                   [pskeys[col // 512]])
                        act(ptb[:, :], psb[:, :], AF.Exp, list(pskeys), [ptkey])
                        op("dve", "tensor_tensor", [ptkey, "maskPC16"], [ptkey],
                           out=ptb[:, :].rearrange("p (a c) -> p a c", a=2),
                           in0=ptb[:, :].rearrange("p (a c) -> p a c", a=2),
                           in1=maskPC16[:, :].unsqueeze(1).to_broadcast([128, 2, 512]), op=ALU.mult)

                    def a_back(gr, itx):
                        hh, qbs, dst, first = gr
                        vcols = slice(0, 65) if hh == 0 else slice(66, 194)
                        M = 65 if hh == 0 else 128
                        rsl = slice(0, 65) if hh == 0 else slice(0, 128)
                        uak = ("Ua", hh)
                        ptb = pt[itx % 2]
                        ptkey = ("pt", itx % 2)
                        po = ps_o[itx % 2]
                        pokey = PS_O_KEYS[itx % 2]
                        for q, (qsl, prev, cur) in enumerate(qbs):
                            for u, (ksl, vi) in enumerate((prev, cur)):
                                col = (2 * q + u) * 128
                                mm(po[0:M, q * 128:(q + 1) * 128], vp[:, vi, vcols], ptb[:, col:col + 128],
                                   u == 0, u == 1, vp_keys(vi) + [ptkey], [pokey])
                        if first:
                            act(dst, po[rsl, :], AF.Copy, [pokey], [uak])
                        else:
                            src = po[rsl, :]
                            if len(dst.shape) == 3:
                                src = src.rearrange("p (r i) -> p r i", r=4)
                            op("dve", "tensor_tensor", [pokey, uak], [uak], out=dst, in0=src, in1=dst, op=ALU.add)

                    ng = len(all_groups)

                    def sched_norms(hh_, pai_):
                        for j_ in range(4):
                            cols = slice(512 * j_, 512 * (j_ + 1))
                            norm_a(hh_, Ua[hh_], cols, [("Ua", hh_)], recs, j_)
                            pending.append((gstep[0] + 3 + 2 * j_, (lambda hh_=hh_, j_=j_, cols=cols, pai_=pai_: norm_b(
                                hh_, Ua[hh_], cols, [("Ua", hh_)], recs, j_, pai_, j_))))

                    for i in range(ng + 1):
                        if i < ng:
                            a_front(all_groups[i], it + i)
                        if i >= 1:
                            a_back(all_groups[i - 1], it + i - 1)
                            if i == ng // 2:
                                sched_norms(0, pai)
                            if i == ng:
                                sched_norms(1, pai)
                        gstep[0] += 1
                        flush_pending()
                    it += ng
                flush_pending(all_=True)
        S.fence()
        checkpoint(4)

        if debug:
            with contextlib.ExitStack() as dstk:
                dtmp = sbt(dstk, "dtmp", [128, 8 * S_OWN], F32)
                op("dve", "tensor_copy", [("mixT", p, j) for p in range(8) for j in range(4)], ["dtmp"],
                   out=dtmp[:, :], in_=mixT[:, :, :].rearrange("p a t -> p (a t)"))
                out_ops.append(dma("sp", dbg["mixT"], dtmp[:, :], ["dtmp"], []))
            S.fence()
            checkpoint(5)

        with contextlib.ExitStack() as p2:
            x1 = sbt(p2, "x1", [128, 16, D], F32)
            h2T = sbt(p2, "h2T", [128, 8, S_OWN], BF16)
            comb = sbt(p2, "comb", [128, 16, 16], F32)
            mixT_keys = [("mixT", p, j) for p in range(8) for j in range(4)]
            with contextlib.ExitStack() as p2a:
                wo16 = sbt(p2a, "wo16", [128, 8, D], BF16)
                xbuf = [sbt(p2a, "xb2_%d" % i, [128, D], F32) for i in range(2)]
                junk32 = sbt(p2a, "junk32", [128, D], F32)
                h2f2 = [sbt(p2a, "h2f%d" % i, [128, D], F32) for i in range(2)]
                h2Tf2 = [sbt(p2a, "h2Tf%d" % i, [128, 8, 128], F32) for i in range(2)]
                wr32 = sbt(p2a, "wr32", [128, 8, 20], F32)
                lg = sbt(p2a, "lg", [128, 16, 20], F32)
                r_gmax = sbt(p2a, "r_gmax", [128, 16], F32)
                r_ng = sbt(p2a, "r_ng", [128, 16], F32)
                r_ge = sbt(p2a, "r_ge", [128, 16, 4], F32)
                r_gsum = sbt(p2a, "r_gsum", [128, 16], F32)
                r_ptop = sbt(p2a, "r_ptop", [128, 16], F32)
                r_oh = sbt(p2a, "r_oh", [128, 16, 4], F32)
                r_em = sbt(p2a, "r_em", [128, 16, 16], F32)
                r_em2 = sbt(p2a, "r_em2", [128, 16, 16], F32)
                r_oh1 = sbt(p2a, "r_oh1", [128, 16, 16], F32)
                r_oh2 = sbt(p2a, "r_oh2", [128, 16, 16], F32)
                r_v1 = sbt(p2a, "r_v1", [128, 16], F32)
                r_v2 = sbt(p2a, "r_v2", [128, 16], F32)
                r_d = sbt(p2a, "r_d", [128, 16], F32)
                r_w1 = sbt(p2a, "r_w1", [128, 16], F32)
                r_w2 = sbt(p2a, "r_w2", [128, 16], F32)
                dma("pool", wo16[:, :, :], w_out.rearrange("(kc p) n -> p kc n", p=128), [], ["wo16"])
                dma("sp", wr32[:, :, :], w_route.rearrange("(kc p) n -> p kc n", p=128), [], ["wr32"])
                dma("sp", gnb[:], ffn_norm.partition_broadcast(128), [], ["gnb"])
                def p2_stage_a(tb):
                    xt = xbuf[tb % 2]
                    xk = ("xb2", tb % 2)
                    dma("sp", xt[:], xkv[S_OWN + tb * 128:S_OWN + (tb + 1) * 128, :], [], [xk])
                    for half in range(2):
                        bk, bkey = banks[4 + half]
                        for kc in range(8):
                            mm(bk, mixT[:, kc, tb * 128:(tb + 1) * 128], wo16[:, kc, half * 512:(half + 1) * 512],
                               kc == 0, kc == 7, mixT_keys + ["wo16"], [bkey])
                        op("dve", "tensor_tensor", [bkey, xk], [("x1", tb, half)], out=x1[:, tb, half * 512:(half + 1) * 512],
                           in0=bk, in1=xt[:, half * 512:(half + 1) * 512], op=ALU.add)
                    x1k = [("x1", tb, 0), ("x1", tb, 1)]
                    rms_stats(x1[:, tb, :], tb, x1k, junk32[:])
                    op("dve", "scalar_tensor_tensor", x1k + [("rstd", tb), "gnb"], [("h2f", tb % 2)], out=h2f2[tb % 2][:],
                       in0=x1[:, tb, :], scalar=rstd[:, tb:tb + 1], in1=gnb[:], op0=ALU.mult, op1=ALU.mult)

                def p2_stage_b(tb):
                    hf = h2f2[tb % 2]
                    psT = ps_s[tb % 2]
                    psTk = PS_S_KEYS[tb % 2]
                    for kc in range(8):
                        S.add("pe", (lambda kc=kc, psT=psT, hf=hf: (lambda e: e.transpose(psT[:, kc * 128:(kc + 1) * 128],
                                                                                         hf[:, kc * 128:(kc + 1) * 128], ident32[:])))(),
                              [("h2f", tb % 2), "ident32"], [psTk[kc // 4]])
                    srcT = psT[:, :].rearrange("p (k t) -> p k t", k=8)
                    hTf = h2Tf2[tb % 2]
                    act(h2T[:, :, tb * 128:(tb + 1) * 128], srcT, AF.Copy, list(psTk), [("h2T", tb // 4)])
                    act(hTf[:, :, :], srcT, AF.Copy, list(psTk), [("h2Tf", tb % 2)])

                def p2_stage_c(tb):
                    hTf = h2Tf2[tb % 2]
                    for kc in range(8):
                        mm(ps_m[:, 0:20], hTf[:, kc, :], wr32[:, kc, :], kc == 0, kc == 7, [("h2Tf", tb % 2), "wr32"], ["b6"])
                    op("dve", "tensor_copy", ["b6"], [("lg", tb)], out=lg[:, tb, :], in_=ps_m[:, 0:20])

                for i in range(18):
                    if i < 16:
                        p2_stage_a(i)
                    if 1 <= i <= 16:
                        p2_stage_b(i - 1)
                    if i >= 2:
                        p2_stage_c(i - 2)
                lgk = [("lg", tb) for tb in range(16)]
                lg_g = lg[:, :, 0:4]
                lg_e = lg[:, :, 4:20]

                def bc(t2, n):
                    return t2[:, :].unsqueeze(2).to_broadcast([128, 16, n])

                op("dve", "tensor_reduce", lgk, ["r_gmax"], out=r_gmax[:, :], in_=lg_g, axis=AX.X, op=ALU.max)
                op("dve", "tensor_tensor", lgk + ["r_gmax"], ["r_ge"], out=r_ge[:, :, :], in0=lg_g, in1=bc(r_gmax, 4),
                   op=ALU.subtract)
                op("dve", "tensor_tensor", lgk + ["r_gmax"], ["r_oh"], out=r_oh[:, :, :], in0=lg_g, in1=bc(r_gmax, 4),
                   op=ALU.is_equal)
                act(r_ge[:, :, :], r_ge[:, :, :], AF.Exp, ["r_ge"], ["r_ge2"])
                op("dve", "tensor_reduce", ["r_ge2"], ["r_gsum"], out=r_gsum[:, :], in_=r_ge[:, :, :], axis=AX.X, op=ALU.add)
                op("dve", "reciprocal", ["r_gsum"], ["r_ptop"], out=r_ptop[:, :], in_=r_gsum[:, :])
                op("dve", "tensor_scalar", ["r_oh"], ["r_oh"], out=r_oh[:, :, :], in0=r_oh[:, :, :], scalar1=BIG,
                   scalar2=-BIG, op0=ALU.mult, op1=ALU.add)
                op("dve", "tensor_tensor", lgk + ["r_oh"], ["r_em"],
                   out=r_em[:, :, :].rearrange("p t (g i) -> p t g i", g=4),
                   in0=lg_e.rearrange("p t (g i) -> p t g i", g=4),
                   in1=r_oh[:, :, :].unsqueeze(3).to_broadcast([128, 16, 4, 4]), op=ALU.add)
                op("dve", "tensor_reduce", ["r_em"], ["r_v1"], out=r_v1[:, :], in_=r_em[:, :, :], axis=AX.X, op=ALU.max)
                op("dve", "tensor_tensor", ["r_em", "r_v1"], ["r_oh1"], out=r_oh1[:, :, :], in0=r_em[:, :, :],
                   in1=bc(r_v1, 16), op=ALU.is_equal)
                op("dve", "scalar_tensor_tensor", ["r_oh1", "r_em"], ["r_em2"], out=r_em2[:, :, :], in0=r_oh1[:, :, :],
                   scalar=-BIG, in1=r_em[:, :, :], op0=ALU.mult, op1=ALU.add)
                op("dve", "tensor_reduce", ["r_em2"], ["r_v2"], out=r_v2[:, :], in_=r_em2[:, :, :], axis=AX.X, op=ALU.max)
                op("dve", "tensor_tensor", ["r_em2", "r_v2"], ["r_oh2"], out=r_oh2[:, :, :], in0=r_em2[:, :, :],
                   in1=bc(r_v2, 16), op=ALU.is_equal)
                op("dve", "tensor_tensor", ["r_v1", "r_v2"], ["r_d"], out=r_d[:, :], in0=r_v2[:, :], in1=r_v1[:, :],
                   op=ALU.subtract)
                act(r_d[:, :], r_d[:, :], AF.Exp, ["r_d"], ["r_d2"])
                op("dve", "tensor_scalar", ["r_d2"], ["r_w1"], out=r_w1[:, :], in0=r_d[:, :], scalar1=1.0, scalar2=None,
                   op0=ALU.add)
                op("dve", "reciprocal", ["r_w1"], ["r_w1"], out=r_w1[:, :], in_=r_w1[:, :])
                op("dve", "tensor_tensor", ["r_w1", "r_ptop"], ["r_w1"], out=r_w1[:, :], in0=r_w1[:, :], in1=r_ptop[:, :],
                   op=ALU.mult)
                op("dve", "tensor_tensor", ["r_w1", "r_d2"], ["r_w2"], out=r_w2[:, :], in0=r_w1[:, :], in1=r_d[:, :],
                   op=ALU.mult)
                op("dve", "tensor_tensor", ["r_oh1", "r_w1"], ["r_oh1"], out=r_oh1[:, :, :], in0=r_oh1[:, :, :],
                   in1=bc(r_w1, 16), op=ALU.mult)
                op("dve", "tensor_tensor", ["r_oh2", "r_w2"], ["r_oh2"], out=r_oh2[:, :, :], in0=r_oh2[:, :, :],
                   in1=bc(r_w2, 16), op=ALU.mult)
                op("dve", "tensor_tensor", ["r_oh1", "r_oh2"], ["comb"], out=comb[:, :, :], in0=r_oh1[:, :, :],
                   in1=r_oh2[:, :, :], op=ALU.add)
            S.fence()
            checkpoint(6)
            if debug:
                out_ops.append(dma("sp", dbg["x1"], x1[:, :, :].rearrange("p a t -> p (a t)"), [], []))
                out_ops.append(dma("sp", dbg["comb"], comb[:, :, :].rearrange("p a t -> p (a t)"), [], []))
                S.fence()
                checkpoint(7)

            with contextlib.ExitStack() as p3:
                wg = [sbt(p3, "wg%d" % i, [128, 8, 512], BF16) for i in range(2)]
                wu = [sbt(p3, "wu%d" % i, [128, 8, 512], BF16) for i in range(2)]
                wd = [sbt(p3, "wd%d" % i, [128, 4, D], BF16) for i in range(2)]
                heT = [sbt(p3, "heT%d" % i, [128, 4, 512], BF16) for i in range(2)]
                sg = [sbt(p3, "sg%d" % i, [128, 512], F32) for i in range(2)]
                junk16f = sbt(p3, "junk16f", [128, D], BF16)
                ob = [sbt(p3, "ob%d" % i, [128, D], F32) for i in range(2)]
                dma("sp", gnb[:], final_norm.partition_broadcast(128), [], ["gnb"])
                cnt = [0]

                def moe_load(ex):
                    b = ex % 2
                    dma("pool", wg[b][:, :, :], w_gate[ex].rearrange("(kc p) n -> p kc n", p=128), [], [("wg", b)])
                    dma("pool", wu[b][:, :, :], w_up[ex].rearrange("(kc p) n -> p kc n", p=128), [], [("wu", b)])
                    dma("pool", wd[b][:, :, :], w_down[ex].rearrange("(hc p) n -> p hc n", p=128), [], [("wd", b)])

                def moe_gu(ex, tc):
                    b = ex % 2
                    he = heT[tc % 2]
                    hek = ("heT", tc % 2)
                    for hc in range(4):
                        bg, bgk = banks[(2 * cnt[0]) % 4]
                        bu, buk = banks[(2 * cnt[0] + 1) % 4]
                        cnt[0] += 1
                        for kc in range(8):
                            mm(bg, wg[b][:, kc, hc * 128:(hc + 1) * 128], h2T[:, kc, tc * 512:(tc + 1) * 512],
                               kc == 0, kc == 7, [("wg", b), ("h2T", tc)], [bgk])
                        for kc in range(8):
                            mm(bu, wu[b][:, kc, hc * 128:(hc + 1) * 128], h2T[:, kc, tc * 512:(tc + 1) * 512],
                               kc == 0, kc == 7, [("wu", b), ("h2T", tc)], [buk])
                        sgt = sg[cnt[0] % 2]
                        sgk = ("sg", cnt[0] % 2)
                        act(sgt[:], bg, AF.Silu, [bgk], [sgk])
                        op("dve", "tensor_tensor", [sgk, buk], [(hek, hc)], out=he[:, hc, :], in0=sgt[:], in1=bu,
                           op=ALU.mult)

                def final_block(tb):
                    x1k = [("x1", tb, 0), ("x1", tb, 1)]
                    rms_stats(x1[:, tb, :], 16 + tb, x1k, junk16f[:])
                    o = ob[tb % 2]
                    ok = ("ob", tb % 2)
                    op("dve", "scalar_tensor_tensor", x1k + [("rstd", 16 + tb), "gnb"], [ok], out=o[:], in0=x1[:, tb, :],
                       scalar=rstd[:, 16 + tb:16 + tb + 1], in1=gnb[:], op0=ALU.mult, op1=ALU.mult)
                    out_ops.append(dma("sp", out_d[tb * 128:(tb + 1) * 128, :], o[:], [ok], []))

                def moe_dn(ex, tc):
                    b = ex % 2
                    he = heT[tc % 2]
                    hek = ("heT", tc % 2)
                    for t4 in range(4):
                        tb = tc * 4 + t4
                        for half in range(2):
                            by, byk = banks[4 + (tb * 2 + half) % 2]
                            for hc in range(4):
                                mm(by, he[:, hc, t4 * 128:(t4 + 1) * 128], wd[b][:, hc, half * 512:(half + 1) * 512],
                                   hc == 0, hc == 3, [(hek, h) for h in range(4)] + [("wd", b)], [byk])
                            xs = x1[:, tb, half * 512:(half + 1) * 512]
                            op("dve", "scalar_tensor_tensor", [byk, "comb", ("x1", tb, half)], [("x1", tb, half)],
                               out=xs, in0=by, scalar=comb[:, tb, ex:ex + 1], in1=xs, op0=ALU.mult, op1=ALU.add)
                        if ex == 15:
                            final_block(tb)

                msteps = [(ex, tc) for ex in range(16) for tc in range(4)]
                moe_load(0)
                moe_load(1)
                for i in range(len(msteps) + 1):
                    if i < len(msteps):
                        moe_gu(*msteps[i])
                    if i >= 1:
                        ex_p, tc_p = msteps[i - 1]
                        moe_dn(ex_p, tc_p)
                        if tc_p == 3 and ex_p + 2 < 16:
                            moe_load(ex_p + 2)
    except _Stop:
        pass
    with nc.allow_low_precision("fp32r single-pass K=1 selector matmul broadcasting 1/l (result feeds a bf16 tile)"):
        S.emit(nc, final_wait_ops=out_ops)
    return nc


def _host_consts(c):
    f32 = np.float32
    slots = np.arange(S_ALL)
    pos = (slots if c == 1 else np.maximum(slots - S_OWN, 0)).astype(f32)
    inv_freq = (1.0 / (f32(500000.0) ** (np.arange(0, 16, 2, dtype=f32) / f32(16)))).astype(f32)
    ang = (pos[:, None] * inv_freq[None, :]).astype(f32)
    ang = np.concatenate([ang, ang], axis=-1)
    cos = np.cos(ang).astype(f32).T
    sin = np.sin(ang).astype(f32).T
    cosT = np.ones((128, S_ALL), f32)
    sinT = np.zeros((128, S_ALL), f32)
    for base in (0, 64):
        cosT[base:base + 16] = cos
        sinT[base:base + 16] = sin
    v = np.ones(84, f32)
    if c == 0:
        v[0:16] = 0.0
        for r4 in range(4):
            v[32 + r4 * 5 + 0] = 0.0
        for r16 in range(16):
            v[52 + 2 * r16] = 0.0
    valid = np.tile(v[None, :], (128, 1)).astype(f32)
    k = np.arange(128)[:, None]
    y = np.arange(896)[None, :]
    maskT = (((y - 384) >= k).astype(f32) - 1.0) * 30000.0
    q = np.arange(128)[None, :]
    prev = (q <= k).astype(f32)
    cur = (q >= k).astype(f32)
    maskPC = np.concatenate([prev, cur, prev, cur], axis=1).astype(f32)
    ident = np.eye(128, dtype=f32)
    rotR = np.zeros((128, 128), f32)
    for base in (0, 64):
        for i in range(8):
            rotR[base + i + 8, base + i] = -1.0
            rotR[base + i, base + i + 8] = 1.0
    return dict(rotR=rotR, cosT=cosT, sinT=sinT, valid=valid, maskT=maskT, maskPC=maskPC, ident=ident)


_NC_CACHE = {}


def kernel(x, attn_norm, w_in, b_forget, w_out, ffn_norm, w_group, w_expert,
           w_gate_e, w_up_e, w_down_e, final_norm, _debug=False):
    f32 = np.float32
    x = np.asarray(x, f32)
    B = x.shape[0]
    key = bool(_debug)
    if key not in _NC_CACHE:
        _NC_CACHE[key] = build_nc(debug=_debug)
    nc = _NC_CACHE[key]
    shared = dict(
        w_in=np.ascontiguousarray(np.asarray(w_in, f32)[0]),
        w_out=np.ascontiguousarray(np.asarray(w_out, f32)[0]),
        attn_norm=np.ascontiguousarray(np.asarray(attn_norm, f32)[0][None, :]),
        ffn_norm=np.ascontiguousarray(np.asarray(ffn_norm, f32)[0][None, :]),
        final_norm=np.ascontiguousarray(np.asarray(final_norm, f32)[None, :]),
        b_forget=np.ascontiguousarray(np.asarray(b_forget, f32)[0][:, None]),
        w_route=np.ascontiguousarray(np.concatenate([np.asarray(w_group, f32)[0], np.asarray(w_expert, f32)[0]], axis=1)),
        w_gate=np.ascontiguousarray(np.asarray(w_gate_e, f32)[0]),
        w_up=np.ascontiguousarray(np.asarray(w_up_e, f32)[0]),
        w_down=np.ascontiguousarray(np.asarray(w_down_e, f32)[0]),
    )
    consts = [_host_consts(0), _host_consts(1)]
    in_maps = []
    for core in range(8):
        b, c = core // 2, core % 2
        if c == 1:
            xkv = np.ascontiguousarray(x[b])
        else:
            xkv = np.concatenate([np.zeros((S_OWN, D), f32), x[b, :S_OWN]], axis=0)
        m = dict(shared)
        m.update(consts[c])
        m["xkv"] = xkv
        in_maps.append(m)
    res = run_bass_kernel_spmd(nc, in_maps, core_ids=list(range(8)))
    out = np.empty((B, S_ALL, D), f32)
    for core in range(8):
        b, c = core // 2, core % 2
        out[b, c * S_OWN:(c + 1) * S_OWN] = res.results[core]["out"]
    if _debug:
        return out, res.results
    return out
```

```python
import contextlib
import numpy as np
import concourse.bass as bass
import concourse.mybir as mybir
from concourse.bass_utils import run_bass_kernel_spmd

F32 = mybir.dt.float32
BF16 = mybir.dt.bfloat16
ALU = mybir.AluOpType
AF = mybir.ActivationFunctionType
AX = mybir.AxisListType

ENGS = ("pe", "act", "dve", "pool", "sp")
D = 1024
S_ALL = 4096
S_OWN = 2048
EPS = 1e-6
BIG = 1e30


class Op:
    __slots__ = ("eng", "fn", "reads", "writes", "is_dma", "deps", "signal", "sem", "val")

    def __init__(self, eng, fn, reads, writes, is_dma):
        self.eng = eng
        self.fn = fn
        self.reads = reads
        self.writes = writes
        self.is_dma = is_dma
        self.deps = []
        self.signal = False
        self.sem = None
        self.val = None


class Sched:
    def __init__(self, n_dma_sems=32):
        self.ops = {e: [] for e in ENGS}
        self.last_writer = {}
        self.readers = {}
        self.n_dma_sems = n_dma_sems
        self.dma_rr = 0
        self.dma_rr_pool = 0
        self.dma_last = [None] * n_dma_sems
        self.dma_count = [0] * n_dma_sems
        self.last_on = {e: None for e in ENGS}
        self.pending_fence = {e: [] for e in ENGS}

    def _dep(self, op, prod):
        if prod is None or prod is op:
            return
        if (not prod.is_dma) and (not op.is_dma) and prod.eng == "pe" and op.eng == "pe":
            return
        op.deps.append(prod)

    def add(self, eng, fn, reads=(), writes=(), is_dma=False):
        op = Op(eng, fn, tuple(reads), tuple(writes), is_dma)
        if self.pending_fence[eng]:
            for p in self.pending_fence[eng]:
                if p is not None and not (p.eng == "pe" and eng == "pe" and not p.is_dma and not is_dma):
                    op.deps.append(p)
            self.pending_fence[eng] = []
        for k in op.reads:
            self._dep(op, self.last_writer.get(k))
        for k in op.writes:
            self._dep(op, self.last_writer.get(k))
            for r in self.readers.get(k, ()):
                self._dep(op, r)
        for k in op.reads:
            self.readers.setdefault(k, []).append(op)
        for k in op.writes:
            self.last_writer[k] = op
            self.readers[k] = []
        if is_dma:
            half = self.n_dma_sems // 2
            if eng == "pool":
                k = half + self.dma_rr_pool
                self.dma_rr_pool = (self.dma_rr_pool + 1) % (self.n_dma_sems - half)
            else:
                k = self.dma_rr
                self.dma_rr = (self.dma_rr + 1) % half
            prev = self.dma_last[k]
            if prev is not None:
                op.deps.append(prev)
            self.dma_last[k] = op
            self.dma_count[k] += 1
            op.sem = ("dma", k)
            op.val = 16 * self.dma_count[k]
            op.signal = True
        else:
            self.last_on[eng] = op
        self.ops[eng].append(op)
        return op

    def fence(self):
        prods = [self.last_on[e] for e in ENGS] + list(self.dma_last)
        for e in ENGS:
            self.pending_fence[e] = list(prods)
        self.last_writer = {}
        self.readers = {}

    def emit(self, nc, final_wait_ops=()):
        for e in ENGS:
            for op in self.ops[e]:
                for p in op.deps:
                    if not p.is_dma:
                        p.signal = True
        for e in ENGS:
            c = 0
            for op in self.ops[e]:
                if op.is_dma:
                    continue
                if op.signal:
                    c += 1
                    op.sem = ("eng", e)
                    op.val = c
        with contextlib.ExitStack() as es:
            sems = {}
            for e in ENGS:
                sems[("eng", e)] = es.enter_context(nc.semaphore("s_" + e))
            for k in range(self.n_dma_sems):
                sems[("dma", k)] = es.enter_context(nc.semaphore("s_dma%d" % k))
            block = es.enter_context(nc.Block())
            engmap = {"pe": "tensor", "act": "scalar", "dve": "vector",
                      "pool": "gpsimd", "sp": "sync"}

            def make(e):
                def body(engine):
                    waited = {}
                    for op in self.ops[e]:
                        need = {}
                        for p in op.deps:
                            if p.sem is None:
                                continue
                            if need.get(p.sem, 0) < p.val:
                                need[p.sem] = p.val
                        for s, v in need.items():
                            if waited.get(s, 0) >= v:
                                continue
                            engine.wait_ge(sems[s], v)
                            waited[s] = v
                        ins = op.fn(engine)
                        if op.signal:
                            ins.then_inc(sems[op.sem], 16 if op.is_dma else 1)
                    if e == "sp":
                        for p in final_wait_ops:
                            engine.wait_ge(sems[p.sem], p.val)
                return body

            for e in ENGS:
                getattr(block, engmap[e])(make(e))


class _Stop(Exception):
    pass


def build_nc(debug=False, limit=99):
    nc = bass.Bass("TRN2", target_bir_lowering=False)

    def din(name, shape):
        return nc.dram_tensor(name, shape, F32, kind="ExternalInput").ap()

    xkv = din("xkv", [S_ALL, D])
    w_in = din("w_in", [D, 3080])
    w_out = din("w_out", [D, D])
    attn_norm = din("attn_norm", [1, D])
    ffn_norm = din("ffn_norm", [1, D])
    final_norm = din("final_norm", [1, D])
    b_forget = din("b_forget", [8, 1])
    w_route = din("w_route", [D, 20])
    w_gate = din("w_gate", [16, D, 512])
    w_up = din("w_up", [16, D, 512])
    w_down = din("w_down", [16, 512, D])
    cosT = din("cosT", [128, S_ALL])
    sinT = din("sinT", [128, S_ALL])
    valid_d = din("valid", [128, 84])
    maskT_d = din("maskT", [128, 896])
    maskPC_d = din("maskPC", [128, 512])
    ident_d = din("ident", [128, 128])
    rotR_d = din("rotR", [128, 128])
    out_d = nc.dram_tensor("out", [S_OWN, D], F32, kind="ExternalOutput").ap()
    aug_d = nc.dram_tensor("aug_scratch", [4, 8, S_ALL], BF16, kind="Internal").ap()
    dbg = {}
    if debug:
        dbg["mixT"] = nc.dram_tensor("dbg_mixT", [128, 8 * S_OWN], F32, kind="ExternalOutput").ap()
        dbg["x1"] = nc.dram_tensor("dbg_x1", [128, 16 * D], F32, kind="ExternalOutput").ap()
        dbg["comb"] = nc.dram_tensor("dbg_comb", [128, 16 * 16], F32, kind="ExternalOutput").ap()

    S = Sched()
    out_ops = []

    def op(eng, method, reads, writes, *args, **kwargs):
        return S.add(eng, lambda e: getattr(e, method)(*args, **kwargs), reads, writes)

    def dma(eng, out, in_, reads, writes):
        return S.add(eng, lambda e: e.dma_start(out=out, in_=in_), reads, writes, is_dma=True)

    def mm(out, lhsT, rhs, start, stop, reads, writes):
        return S.add("pe", lambda e: e.matmul(out, lhsT=lhsT, rhs=rhs, start=start, stop=stop), reads, writes)

    def act(out, in_, func, reads, writes, **kw):
        return S.add("act", lambda e: e.activation(out=out, in_=in_, func=func, **kw), reads, writes)

    def checkpoint(n):
        if limit == n:
            S.fence()
            out_ops.append(dma("sp", out_d[0:128, :], gnb_holder[0][:], [], []))
            raise _Stop()

    gnb_holder = []
    try:
      with contextlib.ExitStack() as top:
        def sbt(stack, name, shape, dt):
            return stack.enter_context(nc.sbuf_tensor("sb_" + name, shape, dt))

        ps_s = [top.enter_context(nc.psum_tensor("ps_s%d" % i, [128, 1024], F32)) for i in range(2)]
        ps_o = [top.enter_context(nc.psum_tensor("ps_o%d" % i, [128, 512], F32)) for i in range(2)]
        ps_m = top.enter_context(nc.psum_tensor("ps_m", [128, 512], F32))
        ps_t = top.enter_context(nc.psum_tensor("ps_t", [128, 1024], BF16))
        banks = [(ps_s[0][:, 0:512], "b0"), (ps_s[0][:, 512:1024], "b1"),
                 (ps_s[1][:, 0:512], "b2"), (ps_s[1][:, 512:1024], "b3"),
                 (ps_o[0][:, :], "b4"), (ps_o[1][:, :], "b5"), (ps_m[:, :], "b6")]
        PS_S_KEYS = [("b0", "b1"), ("b2", "b3")]
        PS_O_KEYS = ["b4", "b5"]
        bank_rr = [0]

        bank_override = [None]
        bank7 = (ps_t[:, :].bitcast(F32), "ps_t")

        def next_bank(n=4):
            if bank_override[0] is not None:
                lst = bank_override[0]
                i = bank_rr[0] % len(lst)
                bank_rr[0] += 1
                return lst[i]
            i = bank_rr[0] % n
            bank_rr[0] += 1
            return banks[i]

        ident32 = sbt(top, "ident32", [128, 128], F32)
        ident16 = sbt(top, "ident16", [128, 128], BF16)
        maskT16 = sbt(top, "maskT16", [128, 896], BF16)
        maskPC16 = sbt(top, "maskPC16", [128, 512], BF16)
        valid = sbt(top, "valid", [128, 84], F32)
        gnb = sbt(top, "gnb", [128, D], F32)
        gnb_holder.append(gnb)
        mixT = sbt(top, "mixT", [128, 8, S_OWN], BF16)
        ss = sbt(top, "ss", [128, 32], F32)
        rstd = sbt(top, "rstd", [128, 32], F32)

        dma("sp", ident32[:], ident_d, [], ["ident32"])
        op("dve", "tensor_copy", ["ident32"], ["ident16"], out=ident16[:], in_=ident32[:])
        dma("pool", maskT16[:], maskT_d, [], ["maskT16"])
        dma("pool", maskPC16[:], maskPC_d, [], ["maskPC16"])
        dma("sp", valid[:], valid_d, [], ["valid"])
        dma("sp", gnb[:], attn_norm.partition_broadcast(128), [], ["gnb"])
        ones8 = sbt(top, "ones8", [8, 2], F32)
        op("pool", "memset", [], ["ones8"], ones8[:], 1.0)
        ones32r = sbt(top, "ones32r", [128, 128], mybir.dt.float32r)
        act(ones32r[:, :], ident32[:, :], AF.Identity, ["ident32"], ["ones32r"], scale=0.0, bias=1.0)

        def rms_stats(src_ap, col, srckeys, junk_ap):
            act(junk_ap, src_ap, AF.Square, srckeys, ["junk", ("ss", col)], accum_out=ss[:, col:col + 1])
            act(rstd[:, col:col + 1], ss[:, col:col + 1], AF.Sqrt, [("ss", col)], [("rstd", col)],
                scale=1.0 / D, bias=EPS)
            op("dve", "reciprocal", [("rstd", col)], [("rstd", col)], out=rstd[:, col:col + 1], in_=rstd[:, col:col + 1])

        with contextlib.ExitStack() as att:
            hT = sbt(att, "hT", [128, 8, S_ALL], BF16)
            wqB = sbt(att, "wqB", [128, 8, 128], BF16)
            wkB = sbt(att, "wkB", [128, 8, 128], BF16)
            wvB = sbt(att, "wvB", [128, 8, 128], BF16)
            wf16 = sbt(att, "wf16", [128, 8, 8], BF16)
            for (dst_, col_, n_, key_) in ((wf16, 3072, 8, "wf16"), (wkB, 2048, 128, ("wkB", 0)), (wqB, 1536, 128, ("wqB", 0)),
                                           (wvB, 2560, 128, ("wvB", 0))):
                dma("pool", dst_[:, :, 0:n_], w_in[:, col_:col_ + n_].rearrange("(kc p) n -> p kc n", p=128), [], [key_])
            with contextlib.ExitStack() as p0:
                xbuf = [sbt(p0, "xbuf%d" % i, [128, D], F32) for i in range(6)]
                junk16 = sbt(p0, "junk16", [128, D], BF16)
                xn16 = [sbt(p0, "xn16_%d" % i, [128, D], BF16) for i in range(2)]
                def p0_s1(tb):
                    xt = xbuf[tb % 6]
                    xk = ("xbuf", tb % 6)
                    dma("sp", xt[:], xkv[tb * 128:(tb + 1) * 128, :], [], [xk])
                    rms_stats(xt[:], tb, [xk], junk16[:])

                def p0_s2(tb):
                    xt = xbuf[tb % 6]
                    xk = ("xbuf", tb % 6)
                    xn = xn16[tb % 2]
                    nk = ("xn16", tb % 2)
                    op("dve", "scalar_tensor_tensor", [xk, ("rstd", tb), "gnb"], [nk], out=xn[:], in0=xt[:],
                       scalar=rstd[:, tb:tb + 1], in1=gnb[:], op0=ALU.mult, op1=ALU.mult)
                    tbank = ps_t[:, :] if tb % 2 == 0 else ps_m[:, :].bitcast(BF16)
                    tkey = "ps_t" if tb % 2 == 0 else "b6"
                    for kc in range(8):
                        S.add("pe", (lambda kc=kc, xn=xn, tbank=tbank: (lambda e: e.transpose(tbank[:, kc * 128:(kc + 1) * 128],
                                                                                              xn[:, kc * 128:(kc + 1) * 128], ident16[:])))(),
                              [nk, "ident16"], [tkey])

                def p0_s3(tb):
                    tbank = ps_t[:, :] if tb % 2 == 0 else ps_m[:, :].bitcast(BF16)
                    tkey = "ps_t" if tb % 2 == 0 else "b6"
                    src = tbank.rearrange("p (k t) -> p k t", k=8)
                    dst = hT[:, :, tb * 128:(tb + 1) * 128]
                    if tb % 2 == 0:
                        act(dst, src, AF.Copy, [tkey], [("hT", tb // 4, 0)])
                    else:
                        op("dve", "tensor_copy", [tkey], [("hT", tb // 4, 1)], out=dst, in_=src)

                for i in range(32 + 3):
                    if i < 32:
                        p0_s1(i)
                    if 1 <= i <= 32:
                        p0_s2(i - 1)
                    if 2 <= i - 1 + 1 and 0 <= i - 2 < 32:
                        p0_s3(i - 2)
            S.fence()
            checkpoint(1)

            def hT_keys(chunk):
                return [("hT", chunk, 0), ("hT", chunk, 1)]

            def load_w(dst, col0, ncols, key):
                dma("pool", dst[:, :, 0:ncols],
                    w_in[:, col0:col0 + ncols].rearrange("(kc p) n -> p kc n", p=128), [], [key])

            def proj_fm(wt, wkey, chunk, ncols=128):
                bk, bkey = next_bank()
                for kc in range(8):
                    mm(bk[0:ncols, :], wt[:, kc, 0:ncols], hT[:, kc, chunk * 512:(chunk + 1) * 512],
                       kc == 0, kc == 7, [wkey] + hT_keys(chunk), [bkey])
                return bk, bkey

            def proj_v(wv, vp, tiles, wkey="wv", tag=0):
                for g0 in range(0, len(tiles), 4):
                    grp = tiles[g0:g0 + 4]
                    bk, bkey = next_bank()
                    for i, (vi, tsl, ck) in enumerate(grp):
                        for kc in range(8):
                            mm(bk[:, i * 128:(i + 1) * 128], hT[:, kc, tsl], wv[:, kc, :],
                               kc == 0, kc == 7, [wkey] + ck, [bkey])
                    n = len(grp)
                    vi0 = grp[0][0]
                    assert all(grp[i][0] == vi0 + i for i in range(n))
                    src = bk[:, 0:n * 128].rearrange("p (t c) -> p t c", t=n)
                    act(vp[:, vi0:vi0 + n, 0:64], src[:, :, 0:64], AF.Copy, [bkey], [("vp", tag, vi0, 0)])
                    act(vp[:, vi0:vi0 + n, 130:194], src[:, :, 64:128], AF.Copy, [bkey], [("vp", tag, vi0, 1)])

            def vp_keys(vi, tag=0):
                v0 = (vi // 4) * 4
                return [("vp", tag, v0, 0), ("vp", tag, v0, 1), ("vp_init", tag)]

            def init_vp(vp, ntiles, tag=0):
                vk = ("vp_init", tag)
                op("pool", "memset", [], [vk], vp[:, :, :], 0.0)
                op("dve", "tensor_copy", ["valid", vk], [vk], out=vp[:, :, 64:65],
                   in_=valid[:, 0:ntiles].unsqueeze(2))
                op("dve", "tensor_copy", ["valid", vk], [vk], out=vp[:, :, 66:67],
                   in_=valid[:, 0:ntiles].unsqueeze(2))

            def norm_a(hh, Ua, ua_cols, uakeys, recs, g2):
                row_s = 64 if hh == 0 else 0
                rec32, rhi, rlo = recs
                rs = slice(row_s, row_s + 1)
                op("dve", "reciprocal", uakeys, [("rec32", g2)], out=rec32[g2][rs, :], in_=Ua[rs, ua_cols])

            def norm_b(hh, Ua, ua_cols, uakeys, recs, g2, pair_idx, j):
                row_s = 64 if hh == 0 else 0
                r0 = 0 if hh == 0 else 64
                M = 64 if hh == 0 else 128
                rec32, rhi, rlo = recs
                rs = slice(row_s, row_s + 1)
                mm(ps_m[0:M, :], ones32r[rs, 0:M], rec32[g2][rs, :], True, True, ["ones32r", ("rec32", g2)], ["b6"])
                op("dve", "tensor_tensor", uakeys + ["b6"], [("mixT", pair_idx, j)],
                   out=mixT[r0:r0 + 64, pair_idx, j * 512:(j + 1) * 512], in0=Ua[r0:r0 + 64, ua_cols],
                   in1=ps_m[r0:r0 + 64, :], op=ALU.mult)

            pending = []
            gstep = [0]

            def flush_pending(all_=False):
                pending.sort(key=lambda t: t[0])
                while pending and (all_ or pending[0][0] <= gstep[0]):
                    pending.pop(0)[1]()

            with contextlib.ExitStack() as pb:
                bf = sbt(pb, "bf", [8, 1], F32)
                nbf = sbt(pb, "nbf", [8, 1], F32)
                with contextlib.ExitStack() as pc:
                    chi = sbt(pc, "chi", [8, S_ALL], BF16)
                    clo = sbt(pc, "clo", [8, S_ALL], BF16)
                    nhi = sbt(pc, "nhi", [8, S_OWN], BF16)
                    nlo = sbt(pc, "nlo", [8, S_OWN], BF16)
                    c2 = sbt(pc, "c2", [8, S_ALL], F32)
                    c32 = sbt(pc, "c32", [8, S_ALL], F32)
                    dma("sp", bf[:], b_forget, [], ["bf"])
                    op("dve", "tensor_scalar", ["bf"], ["nbf"], out=nbf[:], in0=bf[:], scalar1=-1.0, scalar2=None,
                       op0=ALU.mult)
                    for chunk in range(8):
                        bk, bkey = proj_fm(wf16, "wf16", chunk, ncols=8)
                        act(c2[:, chunk * 512:(chunk + 1) * 512], bk[0:8, :], AF.Exp, [bkey, "nbf"], [("c2", chunk)],
                            scale=-1.0, bias=nbf[:, 0:1])
                    act(c2[:, :], c2[:, :], AF.Ln, [("c2", c) for c in range(8)], ["c2all"], bias=1.0)
                    op("dve", "tensor_tensor_scan", ["c2all", "ones8"], ["c32"], out=c32[:, :],
                       data0=ones8[0:8, 0:1].to_broadcast([8, S_ALL]), data1=c2[:, :], initial=0.0,
                       op0=ALU.mult, op1=ALU.add)
                    op("dve", "tensor_copy", ["c32"], ["chi"], out=chi[:, :], in_=c32[:, :])
                    op("dve", "tensor_tensor", ["c32", "chi"], ["clo"], out=clo[:, :], in0=c32[:, :], in1=chi[:, :],
                       op=ALU.subtract)
                    op("dve", "tensor_scalar", ["chi"], ["nhi"], out=nhi[:, :], in0=chi[:, S_OWN:S_ALL], scalar1=-1.0,
                       scalar2=None, op0=ALU.mult)
                    op("dve", "tensor_scalar", ["clo"], ["nlo"], out=nlo[:, :], in0=clo[:, S_OWN:S_ALL], scalar1=-1.0,
                       scalar2=None, op0=ALU.mult)
                    dma("sp", aug_d[0], chi[:, :], ["chi"], ["aug_d0"])
                    dma("sp", aug_d[1], clo[:, :], ["clo"], ["aug_d1"])
                    dma("sp", aug_d[2][:, 0:S_OWN], nhi[:, :], ["nhi"], ["aug_d2"])
                    dma("sp", aug_d[3][:, 0:S_OWN], nlo[:, :], ["nlo"], ["aug_d3"])
                S.fence()
                checkpoint(2)
                KTs = [[sbt(pb, "KT%d_%d" % (s_, i), [128, S_ALL], BF16) for i in range(2)] for s_ in range(2)]
                QTs = [[sbt(pb, "QT%d_%d" % (s_, i), [128, S_OWN], BF16) for i in range(2)] for s_ in range(2)]
                vps = [sbt(pb, "vpB%d" % s_, [128, 32, 194], BF16) for s_ in range(2)]
                Ua = [sbt(pb, "UaB%d" % i, [128, 512], F32) for i in range(2)]
                pt = [sbt(pb, "ptB%d" % i, [128, 1024], BF16) for i in range(2)]
                rec32B = [sbt(pb, "rec32B%d" % i, [128, 512], mybir.dt.float32r) for i in range(2)]
                recs = (rec32B, rec32B, rec32B)
                wsets = [(wqB, wkB, wvB),
                         (sbt(pb, "wqB2", [128, 8, 128], BF16), sbt(pb, "wkB2", [128, 8, 128], BF16),
                          sbt(pb, "wvB2", [128, 8, 128], BF16))]
                for s_ in range(2):
                    init_vp(vps[s_], 32, tag=s_)
                    op("pool", "memset", [], [("KT1z", s_)], KTs[s_][1][0:64, :], 0.0)
                    op("pool", "memset", [], [("QT1z", s_)], QTs[s_][1][0:64, :], 0.0)
                    op("pool", "memset", [], [("KT0a", s_)], KTs[s_][0][64:68, :], 1.0)
                    op("pool", "memset", [], [("QT0a", s_)], QTs[s_][0][64:68, :], 1.0)
                    op("pool", "memset", [("KT1z", s_)], [("KT1a", s_)], KTs[s_][1][0:4, :], 1.0)
                    op("pool", "memset", [("QT1z", s_)], [("QT1a", s_)], QTs[s_][1][0:4, :], 1.0)
                checkpoint(20)

                def emit_proj(pbi):
                    s_ = pbi % 2
                    wq, wk, wv = wsets[s_]
                    KT, QT, vp = KTs[s_], QTs[s_], vps[s_]
                    wqk, wkk, wvk = ("wqB", s_), ("wkB", s_), ("wvB", s_)
                    groups = []
                    if pbi > 0:
                        def g_load():
                            load_w(wk, 2048 + 128 * pbi, 128, wkk)
                            load_w(wq, 1536 + 128 * pbi, 128, wqk)
                            load_w(wv, 2560 + 128 * pbi, 128, wvk)
                        groups.append(g_load)
                    for chunk in range(8):
                        def g_k(chunk=chunk):
                            bk, bkey = proj_fm(wk, wkk, chunk)
                            act(KT[0][0:64, chunk * 512:(chunk + 1) * 512], bk[0:64, :], AF.Copy, [bkey], [("KT0", s_, chunk)])
                            op("dve", "tensor_copy", [bkey], [("KT1", s_, chunk)],
                               out=KT[1][64:128, chunk * 512:(chunk + 1) * 512], in_=bk[64:128, :])
                        groups.append(g_k)
                    for j in range(4):
                        def g_q(j=j):
                            bk, bkey = proj_fm(wq, wqk, 4 + j)
                            act(QT[0][0:64, j * 512:(j + 1) * 512], bk[0:64, :], AF.Copy, [bkey], [("QT0", s_, j)], scale=0.125)
                            op("dve", "tensor_scalar", [bkey], [("QT1", s_, j)], out=QT[1][64:128, j * 512:(j + 1) * 512],
                               in0=bk[64:128, :], scalar1=0.125, scalar2=None, op0=ALU.mult)
                        groups.append(g_q)
                    for g0 in range(0, 32, 4):
                        def g_v(g0=g0):
                            proj_v(wv, vp, [(blk, slice(blk * 128, (blk + 1) * 128), hT_keys(blk // 4))
                                            for blk in range(g0, g0 + 4)], wkey=wvk, tag=s_)
                        groups.append(g_v)

                    def g_aug():
                        h0, h1 = 2 * pbi, 2 * pbi + 1
                        dma("sp", KT[0][66:67, :], aug_d[0][h0:h0 + 1, :], ["aug_d0", ("KT0a", s_)], [("KT0aug", s_)])
                        dma("sp", KT[0][67:68, :], aug_d[1][h0:h0 + 1, :], ["aug_d1", ("KT0a", s_)], [("KT0aug2", s_)])
                        dma("sp", QT[0][64:65, :], aug_d[2][h0:h0 + 1, 0:S_OWN], ["aug_d2", ("QT0a", s_)], [("QT0aug", s_)])
                        dma("sp", QT[0][65:66, :], aug_d[3][h0:h0 + 1, 0:S_OWN], ["aug_d3", ("QT0a", s_)], [("QT0aug2", s_)])
                        dma("sp", KT[1][2:3, :], aug_d[0][h1:h1 + 1, :], ["aug_d0", ("KT1a", s_)], [("KT1aug", s_)])
                        dma("sp", KT[1][3:4, :], aug_d[1][h1:h1 + 1, :], ["aug_d1", ("KT1a", s_)], [("KT1aug2", s_)])
                        dma("sp", QT[1][0:1, :], aug_d[2][h1:h1 + 1, 0:S_OWN], ["aug_d2", ("QT1a", s_)], [("QT1aug", s_)])
                        dma("sp", QT[1][1:2, :], aug_d[3][h1:h1 + 1, 0:S_OWN], ["aug_d3", ("QT1a", s_)], [("QT1aug2", s_)])
                    groups.insert(1 if pbi > 0 else 0, g_aug)
                    return groups

                def fox_qk(st, it, s_):
                    hh, j, kb, nkb, g = st
                    kt, qt = KTs[s_][hh], QTs[s_][hh]
                    rows = slice(0, 68) if hh == 0 else slice(0, 128)
                    kaug = [("KT%daug" % hh, s_), ("KT%daug2" % hh, s_), ("KT%da" % hh, s_)] + ([("KT1z", s_)] if hh else [])
                    qaug = [("QT%daug" % hh, s_), ("QT%daug2" % hh, s_), ("QT%da" % hh, s_)] + ([("QT1z", s_)] if hh else [])
                    psb = ps_s[it % 2]
                    pskeys = PS_S_KEYS[it % 2]
                    for u in range(2):
                        k = kb + u
                        dg = k - (16 + 4 * j)
                        if dg >= 0:
                            off = 384 - 128 * dg
                            mm(psb[:, u * 512:(u + 1) * 512], ident16[:, :], maskT16[:, off:off + 512],
                               True, False, ["ident16", "maskT16"], [pskeys[u]])
                        mm(psb[:, u * 512:(u + 1) * 512], kt[rows, k * 128:(k + 1) * 128],
                           qt[rows, j * 512:(j + 1) * 512], dg < 0, True,
                           [("KT%d" % hh, s_, k // 4), ("QT%d" % hh, s_, j)] + kaug + qaug, [pskeys[u]])
                    act(pt[it % 2][:, :], psb[:, :], AF.Exp, list(pskeys), [("pt", it % 2)])

                def fox_pv(st, it, s_, g_base):
                    hh, j, kb, nkb, g = st
                    vp = vps[s_]
                    vcols = slice(0, 65) if hh == 0 else slice(66, 194)
                    M = 65 if hh == 0 else 128
                    gg = g_base + g
                    po = ps_o[gg % 2]
                    pokey = PS_O_KEYS[gg % 2]
                    ptb = pt[it % 2]
                    for u in range(2):
                        k = kb + u
                        mm(po[0:M, :], vp[:, k, vcols], ptb[:, u * 512:(u + 1) * 512], k == 0, k == nkb - 1,
                           vp_keys(k, s_) + [("pt", it % 2)], [pokey])
                    if kb + 2 >= nkb:
                        rsl = slice(0, 65) if hh == 0 else slice(0, 128)
                        op("dve", "tensor_copy", [pokey], [("Ua", gg % 2)], out=Ua[gg % 2][rsl, :], in_=po[rsl, :])
                        norm_a(hh, Ua[gg % 2], slice(0, 512), [("Ua", gg % 2)], recs, gg % 2)
                        return (hh, j, gg)
                    return None

                for g_ in emit_proj(0):
                    g_()
                it = 0
                for pbi in range(4):
                    s_ = pbi % 2
                    queue = emit_proj(pbi + 1) if pbi + 1 < 4 else []
                    steps = []
                    gidx = 0
                    for hh in range(2):
                        for j in range(4):
                            nkb = 16 + 4 * j + 4
                            for kb in range(0, nkb, 2):
                                steps.append((hh, j, kb, nkb, gidx))
                            gidx += 1
                    g_base = 8 * pbi
                    n = len(steps)
                    for i in range(n + 1):
                        if i < n:
                            fox_qk(steps[i], it + i, s_)
                        if i >= 1:
                            fin = fox_pv(steps[i - 1], it + i - 1, s_, g_base)
                            if fin is not None:
                                hh_, j_, gg_ = fin
                                pending.append((gstep[0] + 5, (lambda hh_=hh_, j_=j_, gg_=gg_, pbi=pbi: norm_b(
                                    hh_, Ua[gg_ % 2], slice(0, 512), [("Ua", gg_ % 2)], recs, gg_ % 2, 4 + pbi, j_))))
                        if i >= 2 and queue:
                            bank_override[0] = [bank7, banks[6]]
                            queue.pop(0)()
                            bank_override[0] = None
                        gstep[0] += 1
                        flush_pending()
                    while queue:
                        queue.pop(0)()
                    it += n
                flush_pending(all_=True)
            S.fence()
            checkpoint(3)

            with contextlib.ExitStack() as pa:
                KTa = sbt(pa, "KTa", [128, S_ALL], BF16)
                QTa = sbt(pa, "QTa", [128, S_OWN], BF16)
                vp = sbt(pa, "vpA", [128, 84, 194], BF16)
                Ua = [sbt(pa, "UaA%d" % i, [128, S_OWN], F32) for i in range(2)]
                pt = [sbt(pa, "ptA%d" % i, [128, 1024], BF16) for i in range(2)]
                rec32A = [sbt(pa, "rec32A%d" % i, [128, 512], mybir.dt.float32r) for i in range(4)]
                recs = (rec32A, rec32A, rec32A)
                _wA = tuple(sbt(pa, "w%sA" % n_, [128, 8, 128], BF16) for n_ in ("q", "k", "v"))
                wsetsA = [_wA, _wA]

                def load_wA(pai_):
                    s_ = pai_ % 2
                    load_w(wsetsA[s_][1], 512 + 128 * pai_, 128, ("wkA", 0))
                    load_w(wsetsA[s_][0], 0 + 128 * pai_, 128, ("wqA", 0))
                    load_w(wsetsA[s_][2], 1024 + 128 * pai_, 128, ("wvA", 0))

                load_wA(0)
                cst = [sbt(pa, "cst%d" % i, [128, 512], F32) for i in range(2)]
                snt = [sbt(pa, "snt%d" % i, [128, 512], F32) for i in range(2)]
                tmp1 = sbt(pa, "tmp1", [128, 512], F32)
                tmp2 = sbt(pa, "tmp2", [128, 512], F32)
                init_vp(vp, 84)
                T16 = [sbt(pa, "T16_%d" % i, [128, 512], BF16) for i in range(2)]
                R16 = sbt(pa, "R16", [128, 128], BF16)
                dma("pool", R16[:, :], rotR_d, [], ["R16"])
                v_tiles = [(blk, slice(blk * 128, (blk + 1) * 128), hT_keys(blk // 4)) for blk in range(12, 32)]
                for r4 in range(4):
                    for m in range(3, 8):
                        v_tiles.append((32 + r4 * 5 + (m - 3), slice(512 * m + r4, 512 * (m + 1), 4), hT_keys(m)))
                for r16 in range(16):
                    for n in range(2):
                        v_tiles.append((52 + 2 * r16 + n, slice(2048 * n + r16, 2048 * (n + 1), 16),
                                        [k_ for c in range(4 * n, 4 * n + 4) for k_ in hT_keys(c)]))
                it = 0
                for pai in range(4):
                    wq, wk, wv = wsetsA[pai % 2]
                    wqk_, wkk_, wvk_ = ("wqA", 0), ("wkA", 0), ("wvA", 0)
                    ajobs = []
                    for chunk in range(8):
                        ajobs.append((chunk, True, wk, wkk_, KTa[:, chunk * 512:(chunk + 1) * 512], ("KTa", chunk), 1.0))
                        if chunk >= 4:
                            j = chunk - 4
                            ajobs.append((chunk, False, wq, wqk_, QTa[:, j * 512:(j + 1) * 512], ("QTa", j), 0.125))

                    def a_s1(n):
                        chunk, first, w, wkey, dst, dkey, scl = ajobs[n]
                        if first:
                            dma("sp", cst[chunk % 2][:], cosT[:, chunk * 512:(chunk + 1) * 512], [], [("cst", chunk % 2)])
                            dma("sp", snt[chunk % 2][:], sinT[:, chunk * 512:(chunk + 1) * 512], [], [("snt", chunk % 2)])
                        bT, bTk = proj_fm(w, wkey, chunk)
                        act(T16[n % 2][:, :], bT, AF.Copy, [bTk], [("T16", n % 2)])

                    def a_s2(n):
                        chunk, first, w, wkey, dst, dkey, scl = ajobs[n]
                        ct, st = cst[chunk % 2], snt[chunk % 2]
                        ck, sk = ("cst", chunk % 2), ("snt", chunk % 2)
                        t16, t16k = T16[n % 2], ("T16", n % 2)
                        bR, bRk = next_bank()
                        mm(bR, R16[:, :], t16[:, :], True, True, ["R16", t16k], [bRk])
                        op("dve", "scalar_tensor_tensor", [t16k, ck], ["tmp1"], out=tmp1[:], in0=t16[:, :], scalar=scl,
                           in1=ct[:], op0=ALU.mult, op1=ALU.mult)
                        op("dve", "scalar_tensor_tensor", [bRk, sk], ["tmp2"], out=tmp2[:], in0=bR, scalar=scl,
                           in1=st[:], op0=ALU.mult, op1=ALU.mult)
                        op("pool", "tensor_tensor", ["tmp1", "tmp2"], [dkey], out=dst, in0=tmp1[:], in1=tmp2[:],
                           op=ALU.add)

                    vgroups = [v_tiles[g0:g0 + 4] for g0 in range(0, len(v_tiles), 4)]
                    per_step = -(-len(vgroups) // (len(ajobs) + 1))
                    for n in range(len(ajobs) + 1):
                        if n < len(ajobs):
                            a_s1(n)
                        if n >= 1:
                            a_s2(n - 1)
                        for _ in range(per_step):
                            if vgroups:
                                proj_v(wv, vp, vgroups.pop(0), wkey=wvk_)
                    while vgroups:
                        proj_v(wv, vp, vgroups.pop(0), wkey=wvk_)
                    if pai + 1 < 4:
                        load_wA(pai + 1)
                    flush_pending(all_=True)
                    all_groups = []
                    for hh in range(2):
                        rows = slice(0, 64) if hh == 0 else slice(64, 128)
                        vcols = slice(0, 65) if hh == 0 else slice(66, 194)
                        M = 65 if hh == 0 else 128
                        rsl = slice(0, 65) if hh == 0 else slice(0, 128)
                        ua = Ua[hh]
                        uak = ("Ua", hh)
                        groups = []
                        for gq in range(4):
                            qbs = []
                            for qb in range(4 * gq, 4 * gq + 4):
                                qbs.append((slice(128 * qb, 128 * qb + 128),
                                            (slice(2048 + 128 * (qb - 1), 2048 + 128 * qb), 16 + qb - 1),
                                            (slice(2048 + 128 * qb, 2048 + 128 * (qb + 1)), 16 + qb)))
                            groups.append((qbs, ua[rsl, 512 * gq:512 * (gq + 1)], True))
                        for r4 in range(4):
                            qbs = []
                            for qm in range(4, 8):
                                qbs.append((slice(512 * (qm - 4) + r4, 512 * (qm - 3), 4),
                                            (slice(512 * (qm - 1) + r4, 512 * qm, 4), 32 + r4 * 5 + (qm - 1 - 3)),
                                            (slice(512 * qm + r4, 512 * (qm + 1), 4), 32 + r4 * 5 + (qm - 3))))
                            groups.append((qbs, ua[rsl, r4:S_OWN:4], False))
                        ua16 = ua[rsl, :].rearrange("p (i r) -> p r i", r=16)
                        for g in range(4):
                            qbs = []
                            for r16 in range(4 * g, 4 * g + 4):
                                qbs.append((slice(r16, 2048, 16),
                                            (slice(r16, 2048, 16), 52 + 2 * r16),
                                            (slice(2048 + r16, 4096, 16), 52 + 2 * r16 + 1)))
                            groups.append((qbs, ua16[:, 4 * g:4 * g + 4, :], False))
                        for (qbs, dst, first) in groups:
                            all_groups.append((hh, qbs, dst, first))

                    def a_front(gr, itx):
                        hh, qbs, dst, first = gr
                        rows = slice(0, 64) if hh == 0 else slice(64, 128)
                        psb = ps_s[itx % 2]
                        pskeys = PS_S_KEYS[itx % 2]
                        ptb = pt[itx % 2]
                        ptkey = ("pt", itx % 2)
                        for q, (qsl, prev, cur) in enumerate(qbs):
                            for u, (ksl, vi) in enumerate((prev, cur)):
                                col = (2 * q + u) * 128
                                mm(psb[:, col:col + 128], KTa[rows, ksl], QTa[rows, qsl], True, True,
                                   [("KTa", c) for c in range(8)] + [("QTa", c) for c in range(4)],
                                   [pskeys[col // 512]])
                        act(ptb[:, :], psb[:, :], AF.Exp, list(pskeys), [ptkey])
                        op("dve", "tensor_tensor", [ptkey, "maskPC16"], [ptkey],
                           out=ptb[:, :].rearrange("p (a c) -> p a c", a=2),
                           in0=ptb[:, :].rearrange("p (a c) -> p a c", a=2),
                           in1=maskPC16[:, :].unsqueeze(1).to_broadcast([128, 2, 512]), op=ALU.mult)

                    def a_back(gr, itx):
                        hh, qbs, dst, first = gr
                        vcols = slice(0, 65) if hh == 0 else slice(66, 194)
                        M = 65 if hh == 0 else 128
                        rsl = slice(0, 65) if hh == 0 else slice(0, 128)
                        uak = ("Ua", hh)
                        ptb = pt[itx % 2]
                        ptkey = ("pt", itx % 2)
                        po = ps_o[itx % 2]
                        pokey = PS_O_KEYS[itx % 2]
                        for q, (qsl, prev, cur) in enumerate(qbs):
                            for u, (ksl, vi) in enumerate((prev, cur)):
                                col = (2 * q + u) * 128
                                mm(po[0:M, q * 128:(q + 1) * 128], vp[:, vi, vcols], ptb[:, col:col + 128],
                                   u == 0, u == 1, vp_keys(vi) + [ptkey], [pokey])
                        if first:
                            act(dst, po[rsl, :], AF.Copy, [pokey], [uak])
                        else:
                            src = po[rsl, :]
                            if len(dst.shape) == 3:
                                src = src.rearrange("p (r i) -> p r i", r=4)
                            op("dve", "tensor_tensor", [pokey, uak], [uak], out=dst, in0=src, in1=dst, op=ALU.add)

                    ng = len(all_groups)

                    def sched_norms(hh_, pai_):
                        for j_ in range(4):
                            cols = slice(512 * j_, 512 * (j_ + 1))
                            if hh_ == 1:
                                norm_a(hh_, Ua[hh_], cols, [("Ua", hh_)], recs, j_)
                            else:
                                pending.append((gstep[0] + 3 * j_, (lambda hh_=hh_, j_=j_, cols=cols: norm_a(
                                    hh_, Ua[hh_], cols, [("Ua", hh_)], recs, j_))))
                            pending.append((gstep[0] + 3 * j_ + 3, (lambda hh_=hh_, j_=j_, cols=cols, pai_=pai_: norm_b(
                                hh_, Ua[hh_], cols, [("Ua", hh_)], recs, j_, pai_, j_))))

                    for i in range(ng + 1):
                        if i < ng:
                            a_front(all_groups[i], it + i)
                        if i >= 1:
                            a_back(all_groups[i - 1], it + i - 1)
                            if i == ng // 2:
                                sched_norms(0, pai)
                            if i == ng:
                                sched_norms(1, pai)
                        gstep[0] += 1
                        flush_pending()
                    it += ng
                flush_pending(all_=True)
        S.fence()
        checkpoint(4)

        if debug:
            with contextlib.ExitStack() as dstk:
                dtmp = sbt(dstk, "dtmp", [128, 8 * S_OWN], F32)
                op("dve", "tensor_copy", [("mixT", p, j) for p in range(8) for j in range(4)], ["dtmp"],
                   out=dtmp[:, :], in_=mixT[:, :, :].rearrange("p a t -> p (a t)"))
                out_ops.append(dma("sp", dbg["mixT"], dtmp[:, :], ["dtmp"], []))
            S.fence()
            checkpoint(5)

        with contextlib.ExitStack() as p2:
            x1 = sbt(p2, "x1", [128, 16, D], F32)
            h2T = sbt(p2, "h2T", [128, 8, S_OWN], BF16)
            comb = sbt(p2, "comb", [128, 16, 16], F32)
            mixT_keys = [("mixT", p, j) for p in range(8) for j in range(4)]
            with contextlib.ExitStack() as p2a:
                wo16 = sbt(p2a, "wo16", [128, 8, D], BF16)
                xbuf = [sbt(p2a, "xb2_%d" % i, [128, D], F32) for i in range(2)]
                junk32 = sbt(p2a, "junk32", [128, D], F32)
                h2f2 = [sbt(p2a, "h2f%d" % i, [128, D], F32) for i in range(2)]
                h2Tf2 = [sbt(p2a, "h2Tf%d" % i, [128, 8, 128], F32) for i in range(2)]
                wr32 = sbt(p2a, "wr32", [128, 8, 20], F32)
                lg = sbt(p2a, "lg", [128, 16, 20], F32)
                r_gmax = sbt(p2a, "r_gmax", [128, 16], F32)
                r_ng = sbt(p2a, "r_ng", [128, 16], F32)
                r_ge = sbt(p2a, "r_ge", [128, 16, 4], F32)
                r_gsum = sbt(p2a, "r_gsum", [128, 16], F32)
                r_ptop = sbt(p2a, "r_ptop", [128, 16], F32)
                r_oh = sbt(p2a, "r_oh", [128, 16, 4], F32)
                r_em = sbt(p2a, "r_em", [128, 16, 16], F32)
                r_em2 = sbt(p2a, "r_em2", [128, 16, 16], F32)
                r_oh1 = sbt(p2a, "r_oh1", [128, 16, 16], F32)
                r_oh2 = sbt(p2a, "r_oh2", [128, 16, 16], F32)
                r_v1 = sbt(p2a, "r_v1", [128, 16], F32)
                r_v2 = sbt(p2a, "r_v2", [128, 16], F32)
                r_d = sbt(p2a, "r_d", [128, 16], F32)
                r_w1 = sbt(p2a, "r_w1", [128, 16], F32)
                r_w2 = sbt(p2a, "r_w2", [128, 16], F32)
                dma("pool", wo16[:, :, :], w_out.rearrange("(kc p) n -> p kc n", p=128), [], ["wo16"])
                dma("sp", wr32[:, :, :], w_route.rearrange("(kc p) n -> p kc n", p=128), [], ["wr32"])
                dma("sp", gnb[:], ffn_norm.partition_broadcast(128), [], ["gnb"])
                def p2_stage_a(tb):
                    xt = xbuf[tb % 2]
                    xk = ("xb2", tb % 2)
                    dma("sp", xt[:], xkv[S_OWN + tb * 128:S_OWN + (tb + 1) * 128, :], [], [xk])
                    for half in range(2):
                        bk, bkey = banks[4 + half]
                        for kc in range(8):
                            mm(bk, mixT[:, kc, tb * 128:(tb + 1) * 128], wo16[:, kc, half * 512:(half + 1) * 512],
                               kc == 0, kc == 7, mixT_keys + ["wo16"], [bkey])
                        op("dve", "tensor_tensor", [bkey, xk], [("x1", tb, half)], out=x1[:, tb, half * 512:(half + 1) * 512],
                           in0=bk, in1=xt[:, half * 512:(half + 1) * 512], op=ALU.add)
                    x1k = [("x1", tb, 0), ("x1", tb, 1)]
                    rms_stats(x1[:, tb, :], tb, x1k, junk32[:])
                    op("dve", "scalar_tensor_tensor", x1k + [("rstd", tb), "gnb"], [("h2f", tb % 2)], out=h2f2[tb % 2][:],
                       in0=x1[:, tb, :], scalar=rstd[:, tb:tb + 1], in1=gnb[:], op0=ALU.mult, op1=ALU.mult)

                def p2_stage_b(tb):
                    hf = h2f2[tb % 2]
                    psT = ps_s[tb % 2]
                    psTk = PS_S_KEYS[tb % 2]
                    for kc in range(8):
                        S.add("pe", (lambda kc=kc, psT=psT, hf=hf: (lambda e: e.transpose(psT[:, kc * 128:(kc + 1) * 128],
                                                                                         hf[:, kc * 128:(kc + 1) * 128], ident32[:])))(),
                              [("h2f", tb % 2), "ident32"], [psTk[kc // 4]])
                    srcT = psT[:, :].rearrange("p (k t) -> p k t", k=8)
                    hTf = h2Tf2[tb % 2]
                    act(h2T[:, :, tb * 128:(tb + 1) * 128], srcT, AF.Copy, list(psTk), [("h2T", tb // 4)])
                    act(hTf[:, :, :], srcT, AF.Copy, list(psTk), [("h2Tf", tb % 2)])

                def p2_stage_c(tb):
                    hTf = h2Tf2[tb % 2]
                    for kc in range(8):
                        mm(ps_m[:, 0:20], hTf[:, kc, :], wr32[:, kc, :], kc == 0, kc == 7, [("h2Tf", tb % 2), "wr32"], ["b6"])
                    op("dve", "tensor_copy", ["b6"], [("lg", tb)], out=lg[:, tb, :], in_=ps_m[:, 0:20])

                for i in range(18):
                    if i < 16:
                        p2_stage_a(i)
                    if 1 <= i <= 16:
                        p2_stage_b(i - 1)
                    if i >= 2:
                        p2_stage_c(i - 2)
                lgk = [("lg", tb) for tb in range(16)]
                lg_g = lg[:, :, 0:4]
                lg_e = lg[:, :, 4:20]

                def bc(t2, n):
                    return t2[:, :].unsqueeze(2).to_broadcast([128, 16, n])

                op("dve", "tensor_reduce", lgk, ["r_gmax"], out=r_gmax[:, :], in_=lg_g, axis=AX.X, op=ALU.max)
                op("dve", "tensor_tensor", lgk + ["r_gmax"], ["r_ge"], out=r_ge[:, :, :], in0=lg_g, in1=bc(r_gmax, 4),
                   op=ALU.subtract)
                op("dve", "tensor_tensor", lgk + ["r_gmax"], ["r_oh"], out=r_oh[:, :, :], in0=lg_g, in1=bc(r_gmax, 4),
                   op=ALU.is_equal)
                act(r_ge[:, :, :], r_ge[:, :, :], AF.Exp, ["r_ge"], ["r_ge2"])
                op("dve", "tensor_reduce", ["r_ge2"], ["r_gsum"], out=r_gsum[:, :], in_=r_ge[:, :, :], axis=AX.X, op=ALU.add)
                op("dve", "reciprocal", ["r_gsum"], ["r_ptop"], out=r_ptop[:, :], in_=r_gsum[:, :])
                op("dve", "tensor_scalar", ["r_oh"], ["r_oh"], out=r_oh[:, :, :], in0=r_oh[:, :, :], scalar1=BIG,
                   scalar2=-BIG, op0=ALU.mult, op1=ALU.add)
                op("dve", "tensor_tensor", lgk + ["r_oh"], ["r_em"],
                   out=r_em[:, :, :].rearrange("p t (g i) -> p t g i", g=4),
                   in0=lg_e.rearrange("p t (g i) -> p t g i", g=4),
                   in1=r_oh[:, :, :].unsqueeze(3).to_broadcast([128, 16, 4, 4]), op=ALU.add)
                op("dve", "tensor_reduce", ["r_em"], ["r_v1"], out=r_v1[:, :], in_=r_em[:, :, :], axis=AX.X, op=ALU.max)
                op("dve", "tensor_tensor", ["r_em", "r_v1"], ["r_oh1"], out=r_oh1[:, :, :], in0=r_em[:, :, :],
                   in1=bc(r_v1, 16), op=ALU.is_equal)
                op("dve", "scalar_tensor_tensor", ["r_oh1", "r_em"], ["r_em2"], out=r_em2[:, :, :], in0=r_oh1[:, :, :],
                   scalar=-BIG, in1=r_em[:, :, :], op0=ALU.mult, op1=ALU.add)
                op("dve", "tensor_reduce", ["r_em2"], ["r_v2"], out=r_v2[:, :], in_=r_em2[:, :, :], axis=AX.X, op=ALU.max)
                op("dve", "tensor_tensor", ["r_em2", "r_v2"], ["r_oh2"], out=r_oh2[:, :, :], in0=r_em2[:, :, :],
                   in1=bc(r_v2, 16), op=ALU.is_equal)
                op("dve", "tensor_tensor", ["r_v1", "r_v2"], ["r_d"], out=r_d[:, :], in0=r_v2[:, :], in1=r_v1[:, :],
                   op=ALU.subtract)
                act(r_d[:, :], r_d[:, :], AF.Exp, ["r_d"], ["r_d2"])
                op("dve", "tensor_scalar", ["r_d2"], ["r_w1"], out=r_w1[:, :], in0=r_d[:, :], scalar1=1.0, scalar2=None,
                   op0=ALU.add)
                op("dve", "reciprocal", ["r_w1"], ["r_w1"], out=r_w1[:, :], in_=r_w1[:, :])
                op("dve", "tensor_tensor", ["r_w1", "r_ptop"], ["r_w1"], out=r_w1[:, :], in0=r_w1[:, :], in1=r_ptop[:, :],
                   op=ALU.mult)
                op("dve", "tensor_tensor", ["r_w1", "r_d2"], ["r_w2"], out=r_w2[:, :], in0=r_w1[:, :], in1=r_d[:, :],
                   op=ALU.mult)
                op("dve", "tensor_tensor", ["r_oh1", "r_w1"], ["r_oh1"], out=r_oh1[:, :, :], in0=r_oh1[:, :, :],
                   in1=bc(r_w1, 16), op=ALU.mult)
                op("dve", "tensor_tensor", ["r_oh2", "r_w2"], ["r_oh2"], out=r_oh2[:, :, :], in0=r_oh2[:, :, :],
                   in1=bc(r_w2, 16), op=ALU.mult)
                op("dve", "tensor_tensor", ["r_oh1", "r_oh2"], ["comb"], out=comb[:, :, :], in0=r_oh1[:, :, :],
                   in1=r_oh2[:, :, :], op=ALU.add)
            S.fence()
            checkpoint(6)
            if debug:
                out_ops.append(dma("sp", dbg["x1"], x1[:, :, :].rearrange("p a t -> p (a t)"), [], []))
                out_ops.append(dma("sp", dbg["comb"], comb[:, :, :].rearrange("p a t -> p (a t)"), [], []))
                S.fence()
                checkpoint(7)

            with contextlib.ExitStack() as p3:
                wg = [sbt(p3, "wg%d" % i, [128, 8, 512], BF16) for i in range(2)]
                wu = [sbt(p3, "wu%d" % i, [128, 8, 512], BF16) for i in range(2)]
                wd = [sbt(p3, "wd%d" % i, [128, 4, D], BF16) for i in range(2)]
                heT = [sbt(p3, "heT%d" % i, [128, 4, 512], BF16) for i in range(2)]
                sg = [sbt(p3, "sg%d" % i, [128, 512], F32) for i in range(2)]
                junk16f = sbt(p3, "junk16f", [128, D], BF16)
                ob = [sbt(p3, "ob%d" % i, [128, D], F32) for i in range(2)]
                dma("sp", gnb[:], final_norm.partition_broadcast(128), [], ["gnb"])
                cnt = [0]

                def moe_load(ex):
                    b = ex % 2
                    dma("pool", wg[b][:, :, :], w_gate[ex].rearrange("(kc p) n -> p kc n", p=128), [], [("wg", b)])
                    dma("pool", wu[b][:, :, :], w_up[ex].rearrange("(kc p) n -> p kc n", p=128), [], [("wu", b)])
                    dma("pool", wd[b][:, :, :], w_down[ex].rearrange("(hc p) n -> p hc n", p=128), [], [("wd", b)])

                def moe_gu(ex, tc):
                    b = ex % 2
                    he = heT[tc % 2]
                    hek = ("heT", tc % 2)
                    for hc in range(4):
                        bg, bgk = banks[(2 * cnt[0]) % 4]
                        bu, buk = banks[(2 * cnt[0] + 1) % 4]
                        cnt[0] += 1
                        for kc in range(8):
                            mm(bg, wg[b][:, kc, hc * 128:(hc + 1) * 128], h2T[:, kc, tc * 512:(tc + 1) * 512],
                               kc == 0, kc == 7, [("wg", b), ("h2T", tc)], [bgk])
                        for kc in range(8):
                            mm(bu, wu[b][:, kc, hc * 128:(hc + 1) * 128], h2T[:, kc, tc * 512:(tc + 1) * 512],
                               kc == 0, kc == 7, [("wu", b), ("h2T", tc)], [buk])
                        sgt = sg[cnt[0] % 2]
                        sgk = ("sg", cnt[0] % 2)
                        act(sgt[:], bg, AF.Silu, [bgk], [sgk])
                        op("dve", "tensor_tensor", [sgk, buk], [(hek, hc)], out=he[:, hc, :], in0=sgt[:], in1=bu,
                           op=ALU.mult)

                def final_block(tb):
                    x1k = [("x1", tb, 0), ("x1", tb, 1)]
                    rms_stats(x1[:, tb, :], 16 + tb, x1k, junk16f[:])
                    o = ob[tb % 2]
                    ok = ("ob", tb % 2)
                    op("dve", "scalar_tensor_tensor", x1k + [("rstd", 16 + tb), "gnb"], [ok], out=o[:], in0=x1[:, tb, :],
                       scalar=rstd[:, 16 + tb:16 + tb + 1], in1=gnb[:], op0=ALU.mult, op1=ALU.mult)
                    out_ops.append(dma("sp", out_d[tb * 128:(tb + 1) * 128, :], o[:], [ok], []))

                def moe_dn(ex, tc):
                    b = ex % 2
                    he = heT[tc % 2]
                    hek = ("heT", tc % 2)
                    for t4 in range(4):
                        tb = tc * 4 + t4
                        for half in range(2):
                            by, byk = banks[4 + (tb * 2 + half) % 2]
                            for hc in range(4):
                                mm(by, he[:, hc, t4 * 128:(t4 + 1) * 128], wd[b][:, hc, half * 512:(half + 1) * 512],
                                   hc == 0, hc == 3, [(hek, h) for h in range(4)] + [("wd", b)], [byk])
                            xs = x1[:, tb, half * 512:(half + 1) * 512]
                            op("dve", "scalar_tensor_tensor", [byk, "comb", ("x1", tb, half)], [("x1", tb, half)],
                               out=xs, in0=by, scalar=comb[:, tb, ex:ex + 1], in1=xs, op0=ALU.mult, op1=ALU.add)
                        if ex == 15:
                            final_block(tb)

                msteps = [(ex, tc) for ex in range(16) for tc in range(4)]
                moe_load(0)
                moe_load(1)
                for i in range(len(msteps) + 1):
                    if i < len(msteps):
                        moe_gu(*msteps[i])
                    if i >= 1:
                        ex_p, tc_p = msteps[i - 1]
                        moe_dn(ex_p, tc_p)
                        if tc_p == 3 and ex_p + 2 < 16:
                            moe_load(ex_p + 2)
    except _Stop:
        pass
    with nc.allow_low_precision("fp32r single-pass K=1 selector matmul broadcasting 1/l (result feeds a bf16 tile)"):
        S.emit(nc, final_wait_ops=out_ops)
    return nc


def _host_consts(c):
    f32 = np.float32
    slots = np.arange(S_ALL)
    pos = (slots if c == 1 else np.maximum(slots - S_OWN, 0)).astype(f32)
    inv_freq = (1.0 / (f32(500000.0) ** (np.arange(0, 16, 2, dtype=f32) / f32(16)))).astype(f32)
    ang = (pos[:, None] * inv_freq[None, :]).astype(f32)
    ang = np.concatenate([ang, ang], axis=-1)
    cos = np.cos(ang).astype(f32).T
    sin = np.sin(ang).astype(f32).T
    cosT = np.ones((128, S_ALL), f32)
    sinT = np.zeros((128, S_ALL), f32)
    for base in (0, 64):
        cosT[base:base + 16] = cos
        sinT[base:base + 16] = sin
    v = np.ones(84, f32)
    if c == 0:
        v[0:16] = 0.0
        for r4 in range(4):
            v[32 + r4 * 5 + 0] = 0.0
        for r16 in range(16):
            v[52 + 2 * r16] = 0.0
    valid = np.tile(v[None, :], (128, 1)).astype(f32)
    k = np.arange(128)[:, None]
    y = np.arange(896)[None, :]
    maskT = (((y - 384) >= k).astype(f32) - 1.0) * 30000.0
    q = np.arange(128)[None, :]
    prev = (q <= k).astype(f32)
    cur = (q >= k).astype(f32)
    maskPC = np.concatenate([prev, cur, prev, cur], axis=1).astype(f32)
    ident = np.eye(128, dtype=f32)
    rotR = np.zeros((128, 128), f32)
    for base in (0, 64):
        for i in range(8):
            rotR[base + i + 8, base + i] = -1.0
            rotR[base + i, base + i + 8] = 1.0
    return dict(rotR=rotR, cosT=cosT, sinT=sinT, valid=valid, maskT=maskT, maskPC=maskPC, ident=ident)


_NC_CACHE = {}


def kernel(x, attn_norm, w_in, b_forget, w_out, ffn_norm, w_group, w_expert,
           w_gate_e, w_up_e, w_down_e, final_norm, _debug=False):
    f32 = np.float32
    x = np.asarray(x, f32)
    B = x.shape[0]
    key = bool(_debug)
    if key not in _NC_CACHE:
        _NC_CACHE[key] = build_nc(debug=_debug)
    nc = _NC_CACHE[key]
    shared = dict(
        w_in=np.ascontiguousarray(np.asarray(w_in, f32)[0]),
        w_out=np.ascontiguousarray(np.asarray(w_out, f32)[0]),
        attn_norm=np.ascontiguousarray(np.asarray(attn_norm, f32)[0][None, :]),
        ffn_norm=np.ascontiguousarray(np.asarray(ffn_norm, f32)[0][None, :]),
        final_norm=np.ascontiguousarray(np.asarray(final_norm, f32)[None, :]),
        b_forget=np.ascontiguousarray(np.asarray(b_forget, f32)[0][:, None]),
        w_route=np.ascontiguousarray(np.concatenate([np.asarray(w_group, f32)[0], np.asarray(w_expert, f32)[0]], axis=1)),
        w_gate=np.ascontiguousarray(np.asarray(w_gate_e, f32)[0]),
        w_up=np.ascontiguousarray(np.asarray(w_up_e, f32)[0]),
        w_down=np.ascontiguousarray(np.asarray(w_down_e, f32)[0]),
    )
    consts = [_host_consts(0), _host_consts(1)]
    in_maps = []
    for core in range(8):
        b, c = core // 2, core % 2
        if c == 1:
            xkv = np.ascontiguousarray(x[b])
        else:
            xkv = np.concatenate([np.zeros((S_OWN, D), f32), x[b, :S_OWN]], axis=0)
        m = dict(shared)
        m.update(consts[c])
        m["xkv"] = xkv
        in_maps.append(m)
    res = run_bass_kernel_spmd(nc, in_maps, core_ids=list(range(8)))
    out = np.empty((B, S_ALL, D), f32)
    for core in range(8):
        b, c = core // 2, core % 2
        out[b, c * S_OWN:(c + 1) * S_OWN] = res.results[core]["out"]
    if _debug:
        return out, res.results
    return out
```
